# Optimizing a Trainium2 kernel written in Bass

```python
import math
import jax
import jax.numpy as jnp
from jax import lax
import numpy as np

D_MODEL = 1024
BATCH = 1
SEQ = 16384
DEPTH = 4

GRID_W = 64
CTX_LEN = 256
N_MIXERS = 4
N_LRU = (DEPTH + 3) // N_MIXERS
N_RWKV = (DEPTH + 2) // N_MIXERS
N_RET = (DEPTH + 1) // N_MIXERS
N_HGRN = DEPTH // N_MIXERS
DEEPNORM_ALPHA = (2.0 * DEPTH) ** 0.25
DEEPNORM_BETA = (8.0 * DEPTH) ** -0.25
LN_EPS = 1e-5

LRU_WIDTH = D_MODEL
LRU_BLOCKS = 16
LRU_BLOCK = LRU_WIDTH // LRU_BLOCKS
LRU_CONV = 4
LRU_C = 8.0

RWKV_HEAD = 64
RWKV_HEADS = D_MODEL // RWKV_HEAD
RWKV_W_LORA = 64
RWKV_A_LORA = 64
RWKV_G_LORA = 128
RWKV_DECAY_SCALE = math.exp(-0.5)
RWKV_GN_EPS = 64e-5

RET_HEADS = 4
RET_QK = D_MODEL // RET_HEADS
RET_V = 2 * RET_QK
RET_CHUNK = 128
ROPE_BASE = 10000.0

HGRN_HEADS = 8
HGRN_EXPAND = D_MODEL // HGRN_HEADS
HGRN_HEAD_V = D_MODEL // HGRN_HEADS
HGRN_CHUNK = 64

N_EXPERTS = 64
TOP_K = 8
N_GROUPS = 8
TOPK_GROUPS = 4
EXPERT_DIM = 256
SHARED_DIM = 256
ROUTED_SCALE = 2.5
MOE_BLOCK = 128

kernel_name = 'hybrid_lru_rwkv7_retnet_hgrn2_moe_trunk'


def _layer_norm(h, g, b):
    hf = h.astype(jnp.float32)
    mu = jnp.mean(hf, axis=-1, keepdims=True)
    var = jnp.mean(jnp.square(hf - mu), axis=-1, keepdims=True)
    return ((hf - mu) * lax.rsqrt(var + LN_EPS)).astype(h.dtype) * g + b


def _head_norm(o, eps):
    of = o.astype(jnp.float32)
    mu = jnp.mean(of, axis=-1, keepdims=True)
    var = jnp.mean(jnp.square(of - mu), axis=-1, keepdims=True)
    return ((of - mu) * lax.rsqrt(var + eps)).astype(o.dtype)


def _head_rms(o, eps):
    of = o.astype(jnp.float32)
    return (of * lax.rsqrt(jnp.mean(jnp.square(of), axis=-1, keepdims=True) + eps)).astype(o.dtype)


def _modulate(h, shift, scale):
    return h * (1.0 + scale) + shift


def _heads(z, n):
    return z.reshape(*z.shape[:-1], n, z.shape[-1] // n)


def _merge(z):
    return z.reshape(*z.shape[:-2], z.shape[-2] * z.shape[-1])


def _rev(z, d):
    return z[:, ::-1] if d == 1 else z


def _to_blocks(z, size):
    b, t, h, f = z.shape
    return z.reshape(b, t // size, size, h, f).transpose(1, 0, 3, 2, 4)


def _from_blocks(z):
    n, b, h, size, f = z.shape
    return z.transpose(1, 0, 3, 2, 4).reshape(b, n * size, h, f)


def _dwconv_centred(z, w, b):
    k = w.shape[0]
    y = lax.conv_general_dilated(z, w[:, None, :], window_strides=(1,), padding=[((k - 1) // 2, k // 2)], dimension_numbers=('NWC', 'WIO', 'NWC'), feature_group_count=z.shape[-1])
    return y + b


def _rope(z, cos, sin):
    half = z.shape[-1] // 2
    rot = jnp.concatenate([-z[..., half:], z[..., :half]], axis=-1)
    return z * cos[None, :, None, :] + rot * sin[None, :, None, :]


def _lru_gates(xc, gate_w, gate_b, lam):
    b, t, w = xc.shape
    xb = xc.reshape(b, t, LRU_BLOCKS, LRU_BLOCK)
    gates = jax.nn.sigmoid(jnp.einsum('btni,gnij->gbtnj', xb, gate_w).reshape(2, b, t, w) + gate_b[:, None, None, :])
    log_a = -LRU_C * gates[0] * jax.nn.softplus(-lam)
    return jnp.exp(log_a), jnp.sqrt(1.0 - jnp.exp(2.0 * log_a)) * (gates[1] * xc)


def _linear_scan(a, bx, h0, reverse):
    def combine(e1, e2):
        return e1[0] * e2[0], e2[0] * e1[1] + e2[1]
    a_cum, b_cum = lax.associative_scan(combine, (a, bx), reverse=reverse, axis=1)
    return a_cum * h0[:, None, :] + b_cum


def _lru_mixer(u_ctx, u_lat, w_in, conv_w, conv_b, gate_w, gate_b, lam, w_out):
    def branches(u):
        gelu_in, rnn_in = jnp.split(u @ w_in, 2, axis=-1)
        return jax.nn.gelu(gelu_in), _dwconv_centred(rnn_in, conv_w, conv_b)
    g_ctx, x_ctx = branches(u_ctx)
    g_lat, x_lat = branches(u_lat)
    h0 = jnp.zeros_like(x_ctx[:, 0])
    hs_ctx, hs_lat = [], []
    for d in range(2):
        a_c, b_c = _lru_gates(x_ctx, gate_w[d], gate_b[d], lam[d])
        a_l, b_l = _lru_gates(x_lat, gate_w[d], gate_b[d], lam[d])
        h_c = _linear_scan(a_c, b_c, h0, d == 1)
        h_end = h_c[:, 0] if d == 1 else h_c[:, -1]
        hs_ctx.append(h_c)
        hs_lat.append(_linear_scan(a_l, b_l, h_end, d == 1))
    return (g_ctx * (hs_ctx[0] + hs_ctx[1])) @ w_out, (g_lat * (hs_lat[0] + hs_lat[1])) @ w_out


def _bi_shift(u):
    half = u.shape[-1] // 2
    prev = jnp.pad(u[:, :-1, :half], ((0, 0), (1, 0), (0, 0)))
    nxt = jnp.pad(u[:, 1:, half:], ((0, 0), (0, 1), (0, 0)))
    return jnp.concatenate([prev, nxt], axis=-1)


def _rwkv_prep(u, mu, w_in, w0, w_l1, w_l2, a0, a_l1, a_l2, g_l1, g_l2, k_k, k_a):
    xm = u[None] + (_bi_shift(u) - u)[None] * mu[:, None, None, :]
    rkv = jnp.einsum('cbtd,cde->cbte', xm[:3], w_in)
    r, k, v = rkv[0], rkv[1], rkv[2]
    d_w = w0[:, None, None, :] + jnp.einsum('zbtl,zld->zbtd', jnp.tanh(jnp.einsum('btd,zdl->zbtl', xm[3], w_l1)), w_l2)
    w = jnp.exp(-RWKV_DECAY_SCALE * jax.nn.sigmoid(d_w))
    a = jax.nn.sigmoid(a0[:, None, None, :] + jnp.einsum('zbtl,zld->zbtd', jnp.einsum('btd,zdl->zbtl', xm[4], a_l1), a_l2))
    g = jax.nn.sigmoid(xm[5] @ g_l1) @ g_l2
    kk = _heads(k * k_k, RWKV_HEADS)
    kk = kk * lax.rsqrt(jnp.sum(jnp.square(kk), axis=-1, keepdims=True) + 1e-12)
    k_t = k[None] * (1.0 + (a - 1.0) * k_a)
    h = RWKV_HEADS
    return _heads(r, h), _heads(v, h), kk, g, _heads(w, h), _heads(k_t, h), _heads(a, h)


def _rwkv_scan(r, w, k, v, kk, a, s0, reverse):
    xs = tuple(jnp.moveaxis(z, 1, 0) for z in (r, w, k, v, kk, a))
    def step(s, inp):
        r_t, w_t, k_t, v_t, kk_t, a_t = inp
        sa = jnp.einsum('bhvk,bhk->bhv', s, kk_t)
        s = s * w_t[:, :, None, :] - sa[..., None] * (kk_t * a_t)[:, :, None, :] + v_t[..., None] * k_t[:, :, None, :]
        return s, jnp.einsum('bhvk,bhk->bhv', s, r_t)
    s_end, o = lax.scan(step, s0, xs, reverse=reverse)
    return jnp.moveaxis(o, 0, 1), s_end


def _rwkv_mixer(u_ctx, u_lat, mu, w_in, w0, w_l1, w_l2, a0, a_l1, a_l2, g_l1, g_l2, k_k, k_a, r_k, ln_g, ln_b, w_out):
    def prep(u):
        return _rwkv_prep(u, mu, w_in, w0, w_l1, w_l2, a0, a_l1, a_l2, g_l1, g_l2, k_k, k_a)
    p_ctx = prep(u_ctx)
    p_lat = prep(u_lat)
    s0 = jnp.zeros((u_ctx.shape[0], RWKV_HEADS, RWKV_HEAD, RWKV_HEAD), u_ctx.dtype)
    o_ctx, o_lat = [], []
    for d in range(2):
        r, v, kk, _, w, k_t, a = p_ctx
        oc, s_c = _rwkv_scan(r, w[d], k_t[d], v, kk, a[d], s0, d == 1)
        r, v, kk, _, w, k_t, a = p_lat
        ol, _ = _rwkv_scan(r, w[d], k_t[d], v, kk, a[d], s_c, d == 1)
        o_ctx.append(oc)
        o_lat.append(ol)
    def readout(p, o):
        r, v, _, g, _, k_t, _ = p
        bonus = jnp.sum(r[None] * k_t * r_k, axis=-1, keepdims=True).sum(axis=0) * v
        y = _merge(_head_norm(o, RWKV_GN_EPS)) * ln_g + ln_b + _merge(bonus)
        return (y * g) @ w_out
    return readout(p_ctx, o_ctx[0] + o_ctx[1]), readout(p_lat, o_lat[0] + o_lat[1])


def _retention_chunks(q, k, v, log_gamma, r0):
    c = RET_CHUNK
    qb, kb, vb = _to_blocks(q, c), _to_blocks(k, c), _to_blocks(v, c)
    pos = jnp.arange(c, dtype=jnp.float32)
    rel = pos[:, None] - pos[None, :]
    lg = log_gamma[:, None, None]
    inner = jnp.where(rel >= 0, jnp.exp(jnp.maximum(rel, 0.0) * lg), 0.0).astype(q.dtype)
    q_dec = jnp.exp((pos + 1.0) * log_gamma[:, None]).astype(q.dtype)[:, :, None]
    k_dec = jnp.exp((c - 1.0 - pos) * log_gamma[:, None]).astype(q.dtype)[:, :, None]
    blk_dec = jnp.exp(c * log_gamma).astype(q.dtype)[:, None, None]
    def step(r, inp):
        q_c, k_c, v_c = inp
        scores = jnp.einsum('bhid,bhjd->bhij', q_c, k_c) * inner
        o = jnp.einsum('bhij,bhjv->bhiv', scores, v_c) + jnp.einsum('bhid,bhdv->bhiv', q_c, r) * q_dec
        r = r * blk_dec + jnp.einsum('bhjd,bhjv->bhdv', k_c * k_dec, v_c)
        return r, o
    r_end, o = lax.scan(step, r0, (qb, kb, vb))
    return _from_blocks(o), r_end


def _retention_mixer(u_ctx, u_lat, rope_cos, rope_sin, w_in, decay_logit, gn_g, gn_b, w_out):
    def proj(u):
        q, k, v, g = jnp.split(u @ w_in, [D_MODEL, 2 * D_MODEL, 2 * D_MODEL + RET_HEADS * RET_V], axis=-1)
        return _heads(q, RET_HEADS), _heads(k, RET_HEADS) * (RET_QK ** -0.5), _heads(v, RET_HEADS), g
    q_c, k_c, v_c, g_c = proj(u_ctx)
    q_l, k_l, v_l, g_l = proj(u_lat)
    q_l = _rope(q_l, rope_cos, rope_sin)
    k_l = _rope(k_l, rope_cos, rope_sin)
    log_gamma = jax.nn.log_sigmoid(decay_logit.astype(jnp.float32))
    r0 = jnp.zeros((u_ctx.shape[0], RET_HEADS, RET_QK, RET_V), u_ctx.dtype)
    o_ctx, o_lat = [], []
    for d in range(2):
        oc, r_c = _retention_chunks(_rev(q_c, d), _rev(k_c, d), _rev(v_c, d), log_gamma[d], r0)
        ol, _ = _retention_chunks(_rev(q_l, d), _rev(k_l, d), _rev(v_l, d), log_gamma[d], r_c)
        o_ctx.append(_rev(oc, d))
        o_lat.append(_rev(ol, d))
    def readout(o, g):
        y = _merge(_head_norm(o, LN_EPS)) * gn_g + gn_b
        return (jax.nn.silu(g) * y) @ w_out
    return readout(o_ctx[0] + o_ctx[1], g_c), readout(o_lat[0] + o_lat[1], g_l)


def _gla_chunks(q, k, v, log_f, s0):
    c = HGRN_CHUNK
    qb, kb, vb = _to_blocks(q, c), _to_blocks(k, c), _to_blocks(v, c)
    cum = jnp.cumsum(_to_blocks(log_f, c), axis=3)
    causal = jnp.tril(jnp.ones((c, c), dtype=bool))[:, :, None]
    def step(s, inp):
        q_c, k_c, v_c, b_c = inp
        diff = b_c[:, :, :, None, :] - b_c[:, :, None, :, :]
        decay = jnp.exp(jnp.where(causal, diff, -jnp.inf))
        scores = jnp.einsum('bhtk,bhsk,bhtsk->bhts', q_c, k_c, decay)
        o = jnp.einsum('bhts,bhsv->bhtv', scores, v_c) + jnp.einsum('bhtk,bhkv->bhtv', q_c * jnp.exp(b_c), s)
        b_last = b_c[:, :, -1:, :]
        s = s * jnp.exp(b_last[:, :, 0, :])[..., None] + jnp.einsum('bhsk,bhsv->bhkv', k_c * jnp.exp(b_last - b_c), v_c)
        return s, o
    s_end, o = lax.scan(step, s0, (qb, kb, vb, cum))
    return _from_blocks(o), s_end


def _hgrn_mixer(u_ctx, u_lat, lb, w_in, b_f, norm_g, w_out):
    def proj(u):
        q, f_fwd, f_bwd, i_in, g = jnp.split(u @ w_in, 5, axis=-1)
        f = lb + (1.0 - lb) * jax.nn.sigmoid(jnp.stack([f_fwd, f_bwd]) + b_f[:, None, None, :])
        return _heads(jax.nn.silu(q), HGRN_HEADS), _heads(i_in, HGRN_HEADS), _heads(f, HGRN_HEADS), g
    q_c, v_c, f_c, g_c = proj(u_ctx)
    q_l, v_l, f_l, g_l = proj(u_lat)
    s0 = jnp.zeros((u_ctx.shape[0], HGRN_HEADS, HGRN_EXPAND, HGRN_HEAD_V), u_ctx.dtype)
    o_ctx, o_lat = [], []
    for d in range(2):
        oc, s_c = _gla_chunks(_rev(q_c, d), _rev(1.0 - f_c[d], d), _rev(v_c, d), _rev(jnp.log(f_c[d]), d), s0)
        ol, _ = _gla_chunks(_rev(q_l, d), _rev(1.0 - f_l[d], d), _rev(v_l, d), _rev(jnp.log(f_l[d]), d), s_c)
        o_ctx.append(_rev(oc, d))
        o_lat.append(_rev(ol, d))
    def readout(o, g):
        return (_merge(_head_rms(o, LN_EPS) * norm_g) * jax.nn.silu(g)) @ w_out
    return readout(o_ctx[0] + o_ctx[1], g_c), readout(o_lat[0] + o_lat[1], g_l)


def _swiglu(z, w_gu, w_down):
    gate, up = jnp.split(z @ w_gu, 2, axis=-1)
    return (jax.nn.silu(gate) * up) @ w_down


def _routed_experts(u, top_idx, top_w, w_gu, w_down):
    n = u.shape[0]
    n_pairs = n * TOP_K
    n_blocks = -(-(n_pairs + N_EXPERTS * (MOE_BLOCK - 1)) // MOE_BLOCK)
    flat_e = top_idx.reshape(-1)
    flat_tok = jnp.arange(n_pairs, dtype=jnp.int32) // TOP_K
    order = jnp.argsort(flat_e)
    e_sorted = flat_e[order]
    counts = jnp.bincount(flat_e, length=N_EXPERTS)
    padded = (counts + MOE_BLOCK - 1) // MOE_BLOCK * MOE_BLOCK
    start = jnp.cumsum(counts) - counts
    padded_end = jnp.cumsum(padded)
    dest = (padded_end - padded)[e_sorted] + jnp.arange(n_pairs, dtype=jnp.int32) - start[e_sorted]
    row_tok = jnp.zeros((n_blocks * MOE_BLOCK,), jnp.int32).at[dest].set(flat_tok[order])
    row_w = jnp.zeros((n_blocks * MOE_BLOCK,), u.dtype).at[dest].set(top_w.reshape(-1)[order])
    block_e = jnp.minimum(jnp.searchsorted(padded_end, jnp.arange(n_blocks, dtype=jnp.int32) * MOE_BLOCK, side='right'), N_EXPERTS - 1)
    def body(acc, inp):
        e, toks, wts = inp
        y = _swiglu(u[toks], w_gu[e], w_down[e])
        return acc.at[toks].add(y * wts[:, None]), None
    acc, _ = lax.scan(body, jnp.zeros_like(u), (block_e, row_tok.reshape(n_blocks, MOE_BLOCK), row_w.reshape(n_blocks, MOE_BLOCK)))
    return acc


def _moe_ffn(u, router_w, router_bias, w_gu, w_down, sh_gu, sh_down):
    n = u.shape[0]
    scores = jax.nn.sigmoid((u @ router_w).astype(jnp.float32))
    choice = scores + router_bias.astype(jnp.float32)
    group_score = lax.top_k(choice.reshape(n, N_GROUPS, N_EXPERTS // N_GROUPS), 2)[0].sum(axis=-1)
    _, top_groups = lax.top_k(group_score, TOPK_GROUPS)
    group_mask = jnp.any(top_groups[:, :, None] == jnp.arange(N_GROUPS)[None, None, :], axis=1)
    expert_mask = jnp.repeat(group_mask, N_EXPERTS // N_GROUPS, axis=1)
    _, top_idx = lax.top_k(jnp.where(expert_mask, choice, -jnp.inf), TOP_K)
    top_w = jnp.take_along_axis(scores, top_idx, axis=1)
    top_w = ROUTED_SCALE * top_w / jnp.sum(top_w, axis=-1, keepdims=True)
    return _routed_experts(u, top_idx, top_w.astype(u.dtype), w_gu, w_down) + _swiglu(u, sh_gu, sh_down)


def setup_inputs(seed: int = 0) -> dict:
    key = jax.random.key(seed)
    ks = iter(jax.random.split(key, 64))
    f32 = jnp.float32
    d = D_MODEL

    def nrm(shape, scale=1.0):
        return jax.random.normal(next(ks), shape, f32) * scale

    def gain(shape):
        return 1.0 + nrm(shape, 0.01)

    lam_u = jax.random.uniform(next(ks), (N_LRU, 2, LRU_WIDTH), f32, 0.9, 0.999)
    p = 5.0 + jnp.arange(RET_HEADS, dtype=f32)
    gam = 1.0 - 2.0 ** (-p)
    ret_logit = jnp.log(gam) - jnp.log1p(-gam)
    hv = RET_HEADS * RET_V
    return {
        'x': nrm((BATCH, SEQ, d)),
        'c': nrm((BATCH, d)),
        'ctx': nrm((BATCH, CTX_LEN, d)),
        'c_ctx': nrm((d,)),
        'ada_w': nrm((DEPTH, d, 6 * d), 0.5 * d ** -0.5),
        'ada_b': nrm((DEPTH, 6 * d), 0.01),
        'post_ln_g': gain((DEPTH, 2, d)),
        'post_ln_b': nrm((DEPTH, 2, d), 0.01),
        'lru_w_in': nrm((N_LRU, d, 2 * LRU_WIDTH), d ** -0.5),
        'lru_conv_w': nrm((N_LRU, LRU_CONV, LRU_WIDTH), LRU_CONV ** -0.5),
        'lru_conv_b': nrm((N_LRU, LRU_WIDTH), 0.01),
        'lru_gate_w': nrm((N_LRU, 2, 2, LRU_BLOCKS, LRU_BLOCK, LRU_BLOCK), LRU_BLOCK ** -0.5),
        'lru_gate_b': nrm((N_LRU, 2, 2, LRU_WIDTH), 0.01),
        'lru_lambda': jnp.log(lam_u) - jnp.log1p(-lam_u),
        'lru_w_out': nrm((N_LRU, LRU_WIDTH, d), DEEPNORM_BETA * LRU_WIDTH ** -0.5),
        'rwkv_mu': jax.random.uniform(next(ks), (N_RWKV, 6, d), f32, 0.0, 1.0),
        'rwkv_w_in': nrm((N_RWKV, 3, d, d), d ** -0.5),
        'rwkv_w0': jax.random.uniform(next(ks), (N_RWKV, 2, d), f32, -4.0, 1.0),
        'rwkv_w_l1': nrm((N_RWKV, 2, d, RWKV_W_LORA), d ** -0.5),
        'rwkv_w_l2': nrm((N_RWKV, 2, RWKV_W_LORA, d), 0.1 * RWKV_W_LORA ** -0.5),
        'rwkv_a0': nrm((N_RWKV, 2, d), 0.1),
        'rwkv_a_l1': nrm((N_RWKV, 2, d, RWKV_A_LORA), d ** -0.5),
        'rwkv_a_l2': nrm((N_RWKV, 2, RWKV_A_LORA, d), 0.1 * RWKV_A_LORA ** -0.5),
        'rwkv_g_l1': nrm((N_RWKV, d, RWKV_G_LORA), d ** -0.5),
        'rwkv_g_l2': nrm((N_RWKV, RWKV_G_LORA, d), RWKV_G_LORA ** -0.5),
        'rwkv_k_k': 1.0 + nrm((N_RWKV, d), 0.1),
        'rwkv_k_a': 1.0 + nrm((N_RWKV, d), 0.1),
        'rwkv_r_k': nrm((N_RWKV, RWKV_HEADS, RWKV_HEAD), 0.1),
        'rwkv_ln_g': gain((N_RWKV, d)),
        'rwkv_ln_b': nrm((N_RWKV, d), 0.01),
        'rwkv_w_out': nrm((N_RWKV, d, d), DEEPNORM_BETA * d ** -0.5),
        'ret_w_in': nrm((N_RET, d, 2 * d + 2 * hv), d ** -0.5),
        'ret_decay': ret_logit[None, None, :] + nrm((N_RET, 2, RET_HEADS), 0.1),
        'ret_gn_g': gain((N_RET, hv)),
        'ret_gn_b': nrm((N_RET, hv), 0.01),
        'ret_w_out': nrm((N_RET, hv, d), DEEPNORM_BETA * hv ** -0.5),
        'hgrn_w_in': nrm((N_HGRN, d, 5 * d), d ** -0.5),
        'hgrn_b_f': nrm((N_HGRN, 2, d), 0.5),
        'hgrn_lb': nrm((DEPTH, d), 0.5),
        'hgrn_norm_g': gain((N_HGRN, HGRN_HEAD_V)),
        'hgrn_w_out': nrm((N_HGRN, d, d), DEEPNORM_BETA * d ** -0.5),
        'moe_router': nrm((DEPTH, d, N_EXPERTS), d ** -0.5),
        'moe_bias': nrm((DEPTH, N_EXPERTS), 0.01),
        'moe_w_gu': nrm((DEPTH, N_EXPERTS, d, 2 * EXPERT_DIM), d ** -0.5),
        'moe_w_down': nrm((DEPTH, N_EXPERTS, EXPERT_DIM, d), DEEPNORM_BETA * EXPERT_DIM ** -0.5),
        'moe_sh_gu': nrm((DEPTH, d, 2 * SHARED_DIM), d ** -0.5),
        'moe_sh_down': nrm((DEPTH, SHARED_DIM, d), DEEPNORM_BETA * SHARED_DIM ** -0.5),
    }


def reference(x, c, ctx, c_ctx, ada_w, ada_b, post_ln_g, post_ln_b,
              lru_w_in, lru_conv_w, lru_conv_b, lru_gate_w, lru_gate_b, lru_lambda, lru_w_out,
              rwkv_mu, rwkv_w_in, rwkv_w0, rwkv_w_l1, rwkv_w_l2, rwkv_a0, rwkv_a_l1, rwkv_a_l2,
              rwkv_g_l1, rwkv_g_l2, rwkv_k_k, rwkv_k_a, rwkv_r_k, rwkv_ln_g, rwkv_ln_b, rwkv_w_out,
              ret_w_in, ret_decay, ret_gn_g, ret_gn_b, ret_w_out,
              hgrn_w_in, hgrn_b_f, hgrn_lb, hgrn_norm_g, hgrn_w_out,
              moe_router, moe_bias, moe_w_gu, moe_w_down, moe_sh_gu, moe_sh_down):
    n_ctx = ctx.shape[1]
    n_lat = x.shape[1]
    rows = n_lat // GRID_W
    pos_row = jnp.repeat(jnp.arange(rows, dtype=jnp.float32), GRID_W)
    pos_col = jnp.tile(jnp.arange(GRID_W, dtype=jnp.float32), rows)
    n_freq = RET_QK // 4
    freqs = ROPE_BASE ** (-jnp.arange(n_freq, dtype=jnp.float32) / n_freq)
    ang = jnp.concatenate([pos_row[:, None] * freqs, pos_col[:, None] * freqs], axis=-1)
    ang = jnp.concatenate([ang, ang], axis=-1)
    rope_cos = jnp.cos(ang).astype(x.dtype)
    rope_sin = jnp.sin(ang).astype(x.dtype)
    lb_cum = jnp.cumsum(jax.nn.softmax(hgrn_lb.astype(jnp.float32), axis=0), axis=0).astype(x.dtype)

    s_lat = jax.nn.silu(c)[:, None, :]
    s_ctx = jax.nn.silu(c_ctx)[None, None, :]
    h_ctx, h_lat = ctx, x
    for i in range(DEPTH):
        kind, j = i % N_MIXERS, i // N_MIXERS
        m_lat = jnp.split(s_lat @ ada_w[i] + ada_b[i], 6, axis=-1)
        m_ctx = jnp.split(s_ctx @ ada_w[i] + ada_b[i], 6, axis=-1)
        u_ctx = _modulate(h_ctx, m_ctx[0], m_ctx[1])
        u_lat = _modulate(h_lat, m_lat[0], m_lat[1])
        if kind == 0:
            y_ctx, y_lat = _lru_mixer(u_ctx, u_lat, lru_w_in[j], lru_conv_w[j], lru_conv_b[j], lru_gate_w[j], lru_gate_b[j], lru_lambda[j], lru_w_out[j])
        elif kind == 1:
            y_ctx, y_lat = _rwkv_mixer(u_ctx, u_lat, rwkv_mu[j], rwkv_w_in[j], rwkv_w0[j], rwkv_w_l1[j], rwkv_w_l2[j], rwkv_a0[j], rwkv_a_l1[j], rwkv_a_l2[j], rwkv_g_l1[j], rwkv_g_l2[j], rwkv_k_k[j], rwkv_k_a[j], rwkv_r_k[j], rwkv_ln_g[j], rwkv_ln_b[j], rwkv_w_out[j])
        elif kind == 2:
            y_ctx, y_lat = _retention_mixer(u_ctx, u_lat, rope_cos, rope_sin, ret_w_in[j], ret_decay[j], ret_gn_g[j], ret_gn_b[j], ret_w_out[j])
        else:
            y_ctx, y_lat = _hgrn_mixer(u_ctx, u_lat, lb_cum[i] - lb_cum[0], hgrn_w_in[j], hgrn_b_f[j], hgrn_norm_g[j], hgrn_w_out[j])
        h_lat = _layer_norm(DEEPNORM_ALPHA * h_lat + m_lat[2] * y_lat, post_ln_g[i, 0], post_ln_b[i, 0])
        moe_args = (moe_router[i], moe_bias[i], moe_w_gu[i], moe_w_down[i], moe_sh_gu[i], moe_sh_down[i])
        if i < DEPTH - 1:
            h_ctx = _layer_norm(DEEPNORM_ALPHA * h_ctx + m_ctx[2] * y_ctx, post_ln_g[i, 0], post_ln_b[i, 0])
            u = jnp.concatenate([_modulate(h_ctx, m_ctx[3], m_ctx[4]), _modulate(h_lat, m_lat[3], m_lat[4])], axis=1)
            y = _moe_ffn(u.reshape(-1, D_MODEL), *moe_args).reshape(u.shape)
            h_ctx = _layer_norm(DEEPNORM_ALPHA * h_ctx + m_ctx[5] * y[:, :n_ctx], post_ln_g[i, 1], post_ln_b[i, 1])
            h_lat = _layer_norm(DEEPNORM_ALPHA * h_lat + m_lat[5] * y[:, n_ctx:], post_ln_g[i, 1], post_ln_b[i, 1])
        else:
            u = _modulate(h_lat, m_lat[3], m_lat[4])
            y = _moe_ffn(u.reshape(-1, D_MODEL), *moe_args).reshape(u.shape)
            h_lat = _layer_norm(DEEPNORM_ALPHA * h_lat + m_lat[5] * y, post_ln_g[i, 1], post_ln_b[i, 1])
    return h_lat
```

```python
import numpy as np
from contextlib import ExitStack
import concourse.bass as bass
import concourse.mybir as mybir
from concourse.bass_utils import run_bass_kernel_spmd

F32 = mybir.dt.float32
BF16 = mybir.dt.bfloat16
I32 = mybir.dt.int32
U32 = mybir.dt.uint32
AF = mybir.ActivationFunctionType
ALU = mybir.AluOpType
AX = mybir.AxisListType

NDSEM = 12


class Prog:
    ENGS = ["sync", "scalar", "vector", "gpsimd", "tensor"]

    def __init__(self, nc):
        self.nc = nc
        self.ops = []
        self.acc = {}
        self.untracked = set()

    def _region(self, ap):
        name = ap.tensor.name
        if name in self.untracked:
            return None
        pairs = ap.ap
        off = int(ap.offset)
        space = str(ap.space)
        if space == "PSUM":
            return (name, 0, 128, 0, 1 << 30)
        if space in ("SB", "PSUM"):
            pstride = pairs[0][0]
            if pstride == 0:
                pstride = 1 << 40
            p0 = off // pstride
            p1 = p0 + pairs[0][1]
            f0 = off % pstride
            ext = 1
            for st, cn in pairs[1:]:
                ext += abs(st) * (cn - 1)
            return (name, p0, p1, f0, f0 + ext)
        else:
            ext = 1
            for st, cn in pairs:
                ext += abs(st) * (cn - 1)
            return (name, 0, 1, off, off + ext)

    @staticmethod
    def _ovl(a, b):
        return a[1] < b[2] and b[1] < a[2] and a[3] < b[4] and b[3] < a[4]

    @staticmethod
    def _covers(a, b):
        return a[1] <= b[1] and a[2] >= b[2] and a[3] <= b[3] and a[4] >= b[4]

    def _add(self, eng, fn, reads, writes, dma=False):
        opid = len(self.ops)
        deps = set()
        rregs = [r for r in (self._region(a) for a in reads if a is not None) if r is not None]
        wregs = [r for r in (self._region(a) for a in writes if a is not None) if r is not None]
        for r in rregs:
            for rec in self.acc.get(r[0], ()):
                if rec[2] and self._ovl(r, rec[0]):
                    deps.add(rec[1])
        for w in wregs:
            for rec in self.acc.get(w[0], ()):
                if self._ovl(w, rec[0]):
                    deps.add(rec[1])
        for w in wregs:
            lst = self.acc.setdefault(w[0], [])
            lst[:] = [rec for rec in lst if not self._covers(w, rec[0])]
            lst.append((w, opid, True, eng, dma))
        for r in rregs:
            lst = self.acc.setdefault(r[0], [])
            lst[:] = [rec for rec in lst if not ((not rec[2]) and rec[3] == eng and not dma and not rec[4] and rec[0] == r)]
            lst.append((r, opid, False, eng, dma))
        deps |= getattr(self, "bar", set())
        self.ops.append(dict(eng=eng, fn=fn, deps=deps, dma=dma))
        return opid

    def dma(self, out, in_, eng="sync", **kw):
        return self._add(eng, lambda e: e.dma_start(out=out, in_=in_, **kw), [in_], [out], dma=True)

    def mm(self, out, lhsT, rhs, start=True, stop=True, **kw):
        return self._add("tensor", lambda e: e.matmul(out, lhsT, rhs, start=start, stop=stop, **kw),
                         [lhsT, rhs] + ([] if start else [out]), [out])

    def transpose(self, out, in_, ident):
        return self._add("tensor", lambda e: e.transpose(out, in_, ident), [in_, ident], [out])

    def act(self, out, in_, func, bias=None, scale=None, accum_out=None, eng="scalar"):
        kw = {}
        rd = [in_]
        if bias is not None:
            kw["bias"] = bias
            if not isinstance(bias, (int, float)):
                rd.append(bias)
        if scale is not None:
            kw["scale"] = scale
            if not isinstance(scale, (int, float)):
                rd.append(scale)
        wr = [out]
        if accum_out is not None:
            kw["accum_out"] = accum_out
            wr.append(accum_out)
        return self._add(eng, lambda e: e.activation(out, in_, func, **kw), rd, wr)

    def tt(self, out, in0, in1, op, eng="vector"):
        return self._add(eng, lambda e: e.tensor_tensor(out, in0, in1, op), [in0, in1], [out])

    def ts(self, out, in0, s1, s2, op0, op1=None, accum_out=None, eng="vector"):
        rd = [in0]
        if not isinstance(s1, (int, float)):
            rd.append(s1)
        if s2 is not None and not isinstance(s2, (int, float)):
            rd.append(s2)
        wr = [out]
        kw = {}
        if accum_out is not None:
            kw["accum_out"] = accum_out
            wr.append(accum_out)
        if op1 is None:
            return self._add(eng, lambda e: e.tensor_scalar(out, in0, s1, None, op0, **kw), rd, wr)
        return self._add(eng, lambda e: e.tensor_scalar(out, in0, s1, s2, op0, op1, **kw), rd, wr)

    def stt(self, out, in0, scalar, in1, op0, op1, eng="vector"):
        rd = [in0, in1]
        if not isinstance(scalar, (int, float)):
            rd.append(scalar)
        return self._add(eng, lambda e: e.scalar_tensor_tensor(out, in0, scalar, in1, op0, op1), rd, [out])

    def copy(self, out, in_, eng="vector"):
        if eng == "scalar":
            return self._add(eng, lambda e: e.copy(out, in_), [in_], [out])
        return self._add(eng, lambda e: e.tensor_copy(out, in_), [in_], [out])

    def memset(self, ap, val, eng="vector"):
        return self._add(eng, lambda e: e.memset(ap, val), [], [ap])

    def reduce(self, out, in_, op, axis=None, eng="vector"):
        axis = axis or AX.X
        return self._add(eng, lambda e: e.tensor_reduce(out, in_, axis, op), [in_], [out])

    def scan(self, out, d0, d1, initial, op0, op1, eng="vector"):
        rd = [d0, d1]
        if not isinstance(initial, (int, float)):
            rd.append(initial)
        return self._add(eng, lambda e: e.tensor_tensor_scan(out, d0, d1, initial, op0, op1), rd, [out])

    def barrier(self):
        last = {}
        for i, op in enumerate(self.ops):
            last[(op["eng"], op["dma"])] = i
        dm = {}
        for i, op in enumerate(self.ops):
            if op["dma"]:
                dm.setdefault(op["eng"], []).append(i)
        b = set(i for (e, isd), i in last.items() if not isd)
        for q, lst in dm.items():
            b.update(lst[-NDSEM:])
        self.bar = b
        self.acc = {}

    def generic(self, eng, fn, reads, writes):
        return self._add(eng, fn, reads, writes)

    def emit(self):
        nc = self.nc
        ops = self.ops
        with ExitStack() as st:
            csem = {e: st.enter_context(nc.semaphore("c_" + e)) for e in ["scalar", "vector", "gpsimd", "tensor"]}
            dsem = {q: [st.enter_context(nc.semaphore("d_%s%d" % (q, i))) for i in range(NDSEM)]
                    for q in ["sync", "gpsimd", "scalar"]}
            cnt = {e: 0 for e in csem}
            dcnt = {q: 0 for q in dsem}
            for op in ops:
                if op["dma"]:
                    q = op["eng"]
                    i = dcnt[q]
                    dcnt[q] += 1
                    op["sem"] = dsem[q][i % NDSEM]
                    op["val"] = 16 * (i // NDSEM + 1)
                    op["prev"] = 16 * (i // NDSEM)
                else:
                    e = op["eng"]
                    cnt[e] += 1
                    op["sem"] = csem[e]
                    op["val"] = cnt[e]
            self.stats = dict(cnt=dict(cnt), dcnt=dict(dcnt), nwait=0)

            def run(engname):
                def f(e):
                    waited = {}
                    for op in ops:
                        if op["eng"] != engname:
                            continue
                        needs = {}
                        for d in op["deps"]:
                            dop = ops[d]
                            if engname == "tensor" and dop["eng"] == "tensor" and not dop["dma"]:
                                continue
                            s = dop["sem"]
                            if needs.get(s, (None, 0))[1] < dop["val"]:
                                needs[s] = (s, dop["val"])
                        if op["dma"] and op["prev"] > 0:
                            s = op["sem"]
                            if needs.get(s, (None, 0))[1] < op["prev"]:
                                needs[s] = (s, op["prev"])
                        for s, v in needs.values():
                            if waited.get(s, 0) < v:
                                e.wait_ge(s, v)
                                waited[s] = v
                                self.stats["nwait"] += 1
                        ins = op["fn"](e)
                        ins.then_inc(op["sem"], 16 if op["dma"] else 1)
                    if engname == "sync":
                        for q in dsem:
                            n = dcnt[q]
                            for i in range(min(n, NDSEM)):
                                uses = (n - 1 - i) // NDSEM + 1
                                e.wait_ge(dsem[q][i], 16 * uses)
                return f

            with nc.Block() as block:
                block.sync(run("sync"))
                block.scalar(run("scalar"))
                block.vector(run("vector"))
                block.gpsimd(run("gpsimd"))
                block.tensor(run("tensor"))


D = 1024
SEQ = 16384
NCTX = 256
NT = SEQ + NCTX
DEPTH = 4
ALPHA = (2.0 * DEPTH) ** 0.25
LN_EPS = 1e-5
NEXP = 64
N_CORES = 1
MOE_EXPERTS_DECL = None
MOE_GROUP_LIMIT = None
DBG = set()
BLOCKS = [(0, NCTX, True)] + [(NCTX + i * 512, 512, False) for i in range(SEQ // 512)]


def _bcast_row(ap_row, nparts, n):
    return bass.AP(ap_row.tensor, int(ap_row.offset), [[0, nparts], [1, n]])


class Ctx:
    pass


def build_program(n_layers=DEPTH, stop_after=None):
    nc = bass.Bass("TRN2", target_bir_lowering=False)
    K = Ctx()
    K.nc = nc
    K.in_names = []

    def din(name, shape, dt=F32):
        K.in_names.append(name)
        return nc.dram_tensor(name, shape, dt, kind="ExternalInput").ap()

    K.din = din
    P = Prog(nc)
    K.P = P
    K.x = din("x", [SEQ, D]); K.ctx = din("ctx", [NCTX, D])
    K.cS = din("cS", [128, 8, 2])
    K.ident_in = din("ident", [128, 128]); K.oh4_in = din("oh4", [4, 4, 128])
    K.out = nc.dram_tensor("out", [SEQ, D], F32, kind="ExternalOutput").ap()
    K.HS = nc.dram_tensor("HS", [NT, D], F32, kind="Internal").ap()
    K.UT = nc.dram_tensor("UT", [D, NT], BF16, kind="Internal").ap()
    K.ZT = nc.dram_tensor("ZT", [2 * D, NT], BF16, kind="Internal").ap()
    K.YS = nc.dram_tensor("YS", [NT, D], F32, kind="Internal").ap()
    K.WT = nc.dram_tensor("WT", [NEXP, NT], F32, kind="Internal").ap()
    K.modrow = nc.dram_tensor("modrow", [DEPTH, 2, 6 * D], F32, kind="Internal").ap()
    K.ps = [nc.alloc_psum_tensor("ps%d" % i, [128, 512], F32) for i in range(8)]
    K.modT = nc.alloc_sbuf_tensor("modT", [128, DEPTH, 48, 2], F32)
    K.sc1p = nc.alloc_sbuf_tensor("sc1p", [128, DEPTH, 8, 2], F32)
    K.ident = nc.alloc_sbuf_tensor("identS", [128, 128], F32)
    K.oh4 = nc.alloc_sbuf_tensor("oh4S", [4, 4, 128], F32)
    P.dma(K.ident[:], K.ident_in)
    P.dma(K.oh4[:], K.oh4_in)

    layers = list(range(n_layers))
    K.scrA = nc.dram_tensor("scrA", [2 * D, NT], F32, kind="Internal").ap()
    K.scrB = nc.dram_tensor("scrB", [D, NT], F32, kind="Internal").ap()
    K.scrC = nc.dram_tensor("scrC", [D, NT], F32, kind="Internal").ap()
    K.ln_g = din("post_ln_g", [DEPTH, 2, D]); K.ln_b = din("post_ln_b", [DEPTH, 2, D])
    K.router = din("moe_router", [DEPTH, D, NEXP]); K.rbias = din("moe_bias", [DEPTH, NEXP])
    K.moe_w_gu = {}; K.moe_w_down = {}; K.moe_sh_gu = {}; K.moe_sh_down = {}; K.w_out = {}
    for l in layers:
        K.moe_w_gu[l] = din("moe_w_gu%d" % l, [MOE_EXPERTS_DECL or NEXP, D, 512])
        K.moe_w_down[l] = din("moe_w_down%d" % l, [MOE_EXPERTS_DECL or NEXP, 256, D])
        K.moe_sh_gu[l] = din("moe_sh_gu%d" % l, [D, 512])
        K.moe_sh_down[l] = din("moe_sh_down%d" % l, [256, D])
    if 0 in layers:
        K.lru_w_in = din("lru_w_in", [1, D, 2 * D]); K.lru_conv_w = din("lru_conv_w", [1, 128, 8, 4])
        K.lru_conv_b = din("lru_conv_b", [1, 128, 8]); K.lru_gate_w = din("lru_gate_w", [1, 2, 2, 8, 128, 128])
        K.lru_gate_b = din("lru_gate_b", [1, 128, 2, 2, 8]); K.lru_lam = din("lru_lam", [1, 128, 2, 8])
        K.w_out[0] = din("lru_w_out", [1, D, D])[0]
    for l in layers:
        if l % 4 != 0:
            declare_mixer_inputs(K, l)
    stage_mod(K, layers)
    stage_epilogue(K, layer=None, sub=None, nxt=(0, "mix"))
    done = False
    for l in layers:
        kind = l % 4
        dz = (mixer_lru, mixer_rwkv, mixer_ret, mixer_hgrn)[kind](K, l)
        last_mix = stop_after == (l, "mix")
        stage_epilogue(K, layer=l, sub="mix", nxt=None if (last_mix and "mixnxt" not in DBG) else (l, "ffn"), dz=dz, to_out=last_mix)
        if last_mix:
            done = True
            break
        if "nomoe" not in DBG:
            stage_moe(K, l)
        last = (l == DEPTH - 1) or stop_after == (l, "ffn")
        stage_epilogue(K, layer=l, sub="ffn", nxt=None if last else (l + 1, "mix"), to_out=last)
        if last:
            done = True
            break
    assert done
    P.emit()
    K.untracked = P.untracked
    return nc, K


def stage_mod(K, layers):
    nc, P, ps = K.nc, K.P, K.ps
    ada_w = K.din("ada_w", [DEPTH, D, 6 * D]); ada_b = K.din("ada_b", [128, DEPTH, 48])
    P.untracked.update(K.in_names)
    with ExitStack() as st:
        S = st.enter_context(nc.sbuf_tensor("S", [128, 8, 2], F32))
        adab = st.enter_context(nc.sbuf_tensor("adab", [128, DEPTH, 48], F32))
        aw = [st.enter_context(nc.sbuf_tensor("aw%d" % i, [128, 8, 512], F32)) for i in range(2)]
        mtr = st.enter_context(nc.sbuf_tensor("mtr", [48, 2, 128], F32))
        P.dma(S[:], K.cS)
        P.dma(adab[:], ada_b)
        P.act(S[:], S[:], AF.Silu)
        i = 0
        for l in layers:
            awv = ada_w[l].rearrange("(k p) n -> p k n", p=128)
            pb = ps[l % 2]
            for nb in range(12):
                buf = aw[i % 2]; i += 1
                P.dma(buf[:], awv[:, :, nb * 512:(nb + 1) * 512])
                for j in range(4):
                    cb = nb * 4 + j
                    for k in range(8):
                        P.mm(pb[:, cb * 2:cb * 2 + 2], buf[:, k, j * 128:(j + 1) * 128], S[:, k, :],
                             start=(k == 0), stop=(k == 7))
            psv = pb[:, 0:96].rearrange("p (c t) -> p c t", t=2)
            for t in range(2):
                P.tt(K.modT[:, l, :, t], psv[:, :, t], adab[:, l, :], ALU.add)
            P.ts(K.sc1p[:, l], K.modT[:, l, 8:16, :], 1.0, None, ALU.add)
            for t in range(2):
                P.transpose(ps[2][0:48, t * 128:(t + 1) * 128], K.modT[:, l, :, t], K.ident[:])
            P.copy(mtr[:].rearrange("c t p -> c (t p)"), ps[2][0:48, 0:256])
            for t in range(2):
                P.dma(K.modrow[l, t].rearrange("(c p) -> c p", p=128), mtr[:, t, :])
    P.barrier()


def stage_epilogue(K, layer, sub, nxt, dz=None, to_out=False, from_hs=False):
    if layer is not None and nxt is not None and "fusedep" not in DBG:
        stage_epilogue(K, layer, sub, None, dz=dz, to_out=to_out)
        stage_epilogue(K, None, None, nxt, from_hs=True)
        return
    nc, P, ps = K.nc, K.P, K.ps
    l = layer
    j0 = 0 if sub == "mix" else 3
    with ExitStack() as st:
        def sb(name, shape, dt=F32):
            K.uid = getattr(K, "uid", 0) + 1
            return st.enter_context(nc.sbuf_tensor("%s_%d" % (name, K.uid), shape, dt))
        if l is not None:
            gateB = sb("gateB", [128, 2, D]); gB = sb("gB", [128, D]); bB = sb("bB", [128, D])
            ln_g = K.ln_g[l]; ln_b = K.ln_b[l]
            si = 0 if sub == "mix" else 1
            for t in range(2):
                P.dma(gateB[:, t, :], _bcast_row(K.modrow[l, t:t + 1, (j0 + 2) * D:(j0 + 3) * D], 128, D))
            P.dma(gB[:], _bcast_row(ln_g[si:si + 1, :], 128, D))
            P.dma(bB[:], _bcast_row(ln_b[si:si + 1, :], 128, D))
            if sub == "mix":
                wo = sb("wo", [128, dz // 128, D], BF16)
                P.dma(wo[:], K.w_out[l].rearrange("(k p) n -> p k n", p=128), eng="gpsimd")
                zt = [sb("zt%d" % i, [128, dz // 128, 128], BF16) for i in range(2)]
                ZTv = K.ZT[0:dz, :].rearrange("(k p) t -> p k t", p=128)
            else:
                yt = [sb("yt%d" % i, [128, D]) for i in range(2)]
        if nxt is not None:
            nl, nsub = nxt
            nj = 0 if nsub == "mix" else 3
            shB = sb("shB", [128, 2, D]); scB = sb("scB", [128, 2, D])
            for t in range(2):
                P.dma(shB[:, t, :], _bcast_row(K.modrow[nl, t:t + 1, nj * D:(nj + 1) * D], 128, D))
                P.dma(scB[:, t, :], _bcast_row(K.modrow[nl, t:t + 1, (nj + 1) * D:(nj + 2) * D], 128, D))
                P.ts(scB[:, t, :], scB[:, t, :], 1.0, None, ALU.add)
            ub = sb("ub", [128, D])
            utb = [sb("utb%d" % i, [128, 8, 128], BF16) for i in range(2)]
            UTv = K.UT.rearrange("(k p) t -> p k t", p=128)
            if nsub == "ffn":
                utf = sb("utf", [128, 8, 128])
                rw = sb("rw", [128, 8, NEXP])
                rbias = sb("rbias", [128, NEXP])
                if "norw" not in DBG:
                    P.dma(rw[:], K.router[nl].rearrange("(k p) e -> p k e", p=128))
                    P.dma(rbias[:], _bcast_row(K.rbias[nl:nl + 1, :], 128, NEXP))
                sco = sb("sco", [128, NEXP]); cho = sb("cho", [128, NEXP]); mc = sb("mc", [128, NEXP])
                m8 = sb("m8", [128, 8, 8]); gs = sb("gs", [128, 8]); g8 = sb("g8", [128, 8]); gm = sb("gm", [128, 8])
                t8 = sb("t8", [128, 8]); wsel = sb("wsel", [128, NEXP]); ssum = sb("ssum", [128, 2])
                wtS = [sb("wtS%d" % i, [NEXP, 128]) for i in range(2)]
        ht = [sb("ht%d" % i, [128, D]) for i in range(2)]
        tt_ = sb("tt", [128, D]); junk = sb("junk", [128, D])
        ot = [sb("ot%d" % i, [128, D]) for i in range(2)]
        st1 = sb("st1", [128, 4])

        last_layer_noctx = (l == DEPTH - 1)
        for ti in range(NT // 128):
            isctx = ti < 2
            if isctx and (last_layer_noctx or (to_out and l is not None) or ("skipctx" in DBG and l is not None)):
                continue
            t = 1 if isctx else 0
            tok0 = ti * 128
            h = ht[ti % 2]; o = ot[ti % 2]
            if l is None:
                src = K.ctx[tok0:tok0 + 128, :] if isctx else K.x[tok0 - NCTX:tok0 - NCTX + 128, :]
                if from_hs:
                    src = K.HS[tok0:tok0 + 128, :]
                P.dma(o[:], src)
            else:
                P.dma(h[:], K.HS[tok0:tok0 + 128, :])
                if sub == "mix":
                    z = zt[ti % 2]
                    P.dma(z[:], ZTv[:, :, tok0:tok0 + 128])
                    nk = dz // 128
                    for nb in range(2):
                        for k in range(nk):
                            P.mm(ps[nb][:, :], z[:, k, :], wo[:, k, nb * 512:(nb + 1) * 512],
                                 start=(k == 0), stop=(k == nk - 1))
                    for nb in range(2):
                        P.tt(tt_[:, nb * 512:(nb + 1) * 512], ps[nb][:, :], gateB[:, t, nb * 512:(nb + 1) * 512], ALU.mult)
                else:
                    y = yt[ti % 2]
                    P.dma(y[:], K.YS[tok0:tok0 + 128, :])
                    P.tt(tt_[:], y[:], gateB[:, t, :], ALU.mult)
                P.stt(tt_[:], h[:], ALPHA, tt_[:], ALU.mult, ALU.add)
                P.reduce(st1[:, 0:1], tt_[:], ALU.add)
                P.ts(st1[:, 1:2], st1[:, 0:1], 1.0 / D, None, ALU.mult)
                P.ts(tt_[:], tt_[:], st1[:, 1:2], None, ALU.subtract)
                P.act(junk[:], tt_[:], AF.Square, accum_out=st1[:, 2:3])
                P.ts(st1[:, 3:4], st1[:, 2:3], 1.0 / D, LN_EPS, ALU.mult, ALU.add)
                P.act(st1[:, 3:4], st1[:, 3:4], AF.Sqrt)
                P._add("vector", lambda e, a=st1[:, 3:4]: e.reciprocal(a, a), [st1[:, 3:4]], [st1[:, 3:4]])
                P.stt(o[:], tt_[:], st1[:, 3:4], gB[:], ALU.mult, ALU.mult)
                P.tt(o[:], o[:], bB[:], ALU.add, eng="gpsimd")
            if to_out:
                P.dma(K.out[tok0 - NCTX:tok0 - NCTX + 128, :], o[:])
                if nxt is None:
                    continue
            if not from_hs:
                P.dma(K.HS[tok0:tok0 + 128, :], o[:])
            if nxt is None:
                continue
            if nl == DEPTH - 1 and nsub == "ffn" and isctx:
                continue
            P.tt(ub[:], o[:], scB[:, t, :], ALU.mult)
            P.tt(ub[:], ub[:], shB[:, t, :], ALU.add, eng="gpsimd")
            utile = utb[ti % 2]
            for hb in range(2):
                for k in range(4):
                    kk = hb * 4 + k
                    P.transpose(ps[2 + hb][:, k * 128:(k + 1) * 128], ub[:, kk * 128:(kk + 1) * 128], K.ident[:])
                if nsub == "ffn":
                    P.copy(utf[:, hb * 4:(hb + 1) * 4, :].rearrange("p k t -> p (k t)"), ps[2 + hb][:, :], eng="scalar")
                    P.copy(utile[:, hb * 4:(hb + 1) * 4, :].rearrange("p k t -> p (k t)"),
                           utf[:, hb * 4:(hb + 1) * 4, :].rearrange("p k t -> p (k t)"), eng="gpsimd")
                else:
                    P.copy(utile[:, hb * 4:(hb + 1) * 4, :].rearrange("p k t -> p (k t)"), ps[2 + hb][:, :], eng="scalar")
            P.dma(UTv[:, :, tok0:tok0 + 128], utile[:])
            if nsub != "ffn" or "norouter" in DBG:
                continue
            for k in range(8):
                P.mm(ps[4][:, 0:NEXP], utf[:, k, :], rw[:, k, :], start=(k == 0), stop=(k == 7))
            P.act(sco[:], ps[4][:, 0:NEXP], AF.Sigmoid)
            P.tt(cho[:], sco[:], rbias[:], ALU.add)
            for g in range(8):
                P._add("vector", lambda e, a=m8[:, g, :], b=cho[:, g * 8:(g + 1) * 8]: e.max(a, b),
                       [cho[:, g * 8:(g + 1) * 8]], [m8[:, g, :]])
            P.tt(gs[:], m8[:, :, 0], m8[:, :, 1], ALU.add)
            P._add("vector", lambda e, a=g8[:], b=gs[:]: e.max(a, b), [gs[:]], [g8[:]])
            P.ts(gm[:], gs[:], g8[:, 3:4], None, ALU.is_ge)
            P.ts(mc[:], cho[:], 2.0, None, ALU.add)
            P.tt(mc[:].rearrange("p (g e) -> p g e", e=8), mc[:].rearrange("p (g e) -> p g e", e=8),
                 gm[:].unsqueeze(2).to_broadcast([128, 8, 8]), ALU.mult)
            P.ts(mc[:], mc[:], -2.0, None, ALU.add)
            P._add("vector", lambda e, a=t8[:], b=mc[:]: e.max(a, b), [mc[:]], [t8[:]])
            P.ts(wsel[:], mc[:], t8[:, 7:8], None, ALU.is_ge)
            P.tt(wsel[:], wsel[:], sco[:], ALU.mult)
            P.reduce(ssum[:, 0:1], wsel[:], ALU.add)
            P._add("vector", lambda e, a=ssum[:, 1:2], b=ssum[:, 0:1]: e.reciprocal(a, b), [ssum[:, 0:1]], [ssum[:, 1:2]])
            P.ts(wsel[:], wsel[:], ssum[:, 1:2], 2.5, ALU.mult, ALU.mult)
            P.transpose(ps[5][0:NEXP, 0:128], wsel[:], K.ident[:])
            w_ = wtS[ti % 2]
            P.copy(w_[:], ps[5][0:NEXP, 0:128], eng="scalar")
            P.dma(K.WT[:, tok0:tok0 + 128], w_[:])
    P.barrier()


def stage_moe(K, l):
    nc, P, ps = K.nc, K.P, K.ps
    w_gu = K.moe_w_gu[l]; w_dn = K.moe_w_down[l]; sh_gu = K.moe_sh_gu[l]; sh_dn = K.moe_sh_down[l]
    noctx = (l == DEPTH - 1)
    groups = [list(range(g * 4, g * 4 + 4)) for g in range(NEXP // 4)] + [[NEXP]]
    if MOE_GROUP_LIMIT is not None:
        groups = groups[:MOE_GROUP_LIMIT] + [[NEXP]]
    with ExitStack() as st:
        def sb(name, shape, dt=F32):
            K.uid = getattr(K, "uid", 0) + 1
            return st.enter_context(nc.sbuf_tensor("%s_%d" % (name, K.uid), shape, dt))
        wgu = [sb("wgu%d" % i, [128, 4, 8, 512], BF16) for i in range(2)]
        wdn = [sb("wdn%d" % i, [128, 4, 2, D], BF16) for i in range(2)]
        uT = [sb("uTm%d" % i, [128, 8, 512], BF16) for i in range(2)]
        wt4 = [sb("wt4%d" % i, [4, 512]) for i in range(2)]
        actT = [sb("actT%d" % i, [128, 4, 2, 512], BF16) for i in range(2)]
        tmp = [sb("tmpm%d" % i, [128, 512]) for i in range(2)]
        yb = [sb("yb%d" % i, [128, D]) for i in range(2)]
        UTv = K.UT.rearrange("(k p) t -> p k t", p=128)
        it = 0
        yi = 0
        for gi, grp in enumerate(groups):
            wg = wgu[gi % 2]; wd = wdn[gi % 2]
            for j, e in enumerate(grp):
                if e < NEXP:
                    P.dma(wg[:, j], w_gu[e].rearrange("(k p) n -> p k n", p=128), eng="gpsimd")
                    P.dma(wd[:, j], w_dn[e].rearrange("(k p) n -> p k n", p=128), eng="gpsimd")
                else:
                    P.dma(wg[:, j], sh_gu.rearrange("(k p) n -> p k n", p=128), eng="gpsimd")
                    P.dma(wd[:, j], sh_dn.rearrange("(k p) n -> p k n", p=128), eng="gpsimd")
            shared = grp[0] == NEXP
            for (s0, n, isctx) in BLOCKS:
                if isctx and noctx:
                    continue
                u = uT[it % 2]; a = actT[it % 2]; w4 = wt4[it % 2]
                it += 1
                P.dma(u[:, :, :n], UTv[:, :, s0:s0 + n])
                if not shared:
                    P.dma(w4[:, :n], K.WT[grp[0]:grp[0] + 4, s0:s0 + n])
                for j, e in enumerate(grp):
                    for ct in range(4):
                        for k in range(8):
                            P.mm(ps[ct][:, :n], wg[:, j, k, ct * 128:(ct + 1) * 128], u[:, k, :n],
                                 start=(k == 0), stop=(k == 7))
                    if not shared:
                        wb = ps[4 + j % 2]
                        P.mm(wb[:, :n], K.oh4[:, j, :], w4[:, :n])
                    for hc in range(2):
                        tm = tmp[hc]
                        P.act(tm[:, :n], ps[hc][:, :n], AF.Silu)
                        if shared:
                            P.tt(a[:, j, hc, :n], tm[:, :n], ps[2 + hc][:, :n], ALU.mult)
                        else:
                            P.tt(tm[:, :n], tm[:, :n], ps[2 + hc][:, :n], ALU.mult)
                            P.tt(a[:, j, hc, :n], tm[:, :n], wb[:, :n], ALU.mult)
                for tl in range(n // 128):
                    tok0 = s0 + tl * 128
                    y = yb[yi % 2]
                    yi += 1
                    if gi > 0:
                        P.dma(y[:], K.YS[tok0:tok0 + 128, :])
                    for half in range(2):
                        pb = ps[6 + half]
                        cnt = 0
                        tot = len(grp) * 2
                        for j in range(len(grp)):
                            for kc in range(2):
                                P.mm(pb[:, :], a[:, j, kc, tl * 128:(tl + 1) * 128], wd[:, j, kc, half * 512:(half + 1) * 512],
                                     start=(cnt == 0), stop=(cnt == tot - 1))
                                cnt += 1
                        if gi > 0:
                            P.tt(y[:, half * 512:(half + 1) * 512], pb[:, :], y[:, half * 512:(half + 1) * 512], ALU.add)
                        else:
                            P.copy(y[:, half * 512:(half + 1) * 512], pb[:, :], eng="scalar")
                    P.dma(K.YS[tok0:tok0 + 128, :], y[:])
    P.barrier()


def mixer_lru(K, l):
    nc, P, ps = K.nc, K.P, K.ps
    j = l // 4
    w_in = K.lru_w_in[j]; conv_w = K.lru_conv_w[j]; conv_b = K.lru_conv_b[j]
    gate_w = K.lru_gate_w[j]; gate_b = K.lru_gate_b[j]; lam = K.lru_lam[j]
    GT = K.scrA[0:D, :]; XT = K.scrB; HF = K.scrC
    ZT = K.ZT
    with ExitStack() as st:
        def sb(name, shape, dt=F32):
            K.uid = getattr(K, "uid", 0) + 1
            return st.enter_context(nc.sbuf_tensor("%s_%d" % (name, K.uid), shape, dt))
        win = sb("win", [128, 8, 2 * D], BF16)
        uT = [sb("uT%d" % i, [128, 8, 512], BF16) for i in range(2)]
        Gb = [sb("Gb%d" % i, [128, 8, 512]) for i in range(2)]
        Xb = [sb("Xb%d" % i, [128, 8, 512]) for i in range(2)]
        t1 = sb("t1", [128, 512]); t2 = sb("t2", [128, 512])
        P.dma(win[:], w_in.rearrange("(k p) n -> p k n", p=128), eng="gpsimd")
        UTv = K.UT.rearrange("(k p) t -> p k t", p=128)
        GTv = GT.rearrange("(k p) t -> p k t", p=128)
        XTv = XT.rearrange("(k p) t -> p k t", p=128)
        for bi, (s0, n, isctx) in enumerate(BLOCKS):
            u = uT[bi % 2]; G = Gb[bi % 2]; X = Xb[bi % 2]
            P.dma(u[:, :, :n], UTv[:, :, s0:s0 + n])
            for ct in range(16):
                pp = ps[2 + ct % 4]
                for k in range(8):
                    P.mm(pp[:, :n], win[:, k, ct * 128:(ct + 1) * 128], u[:, k, :n], start=(k == 0), stop=(k == 7))
                if ct < 8:
                    P.act(t1[:, :n], pp[:, :n], AF.Square)
                    P.ts(t1[:, :n], t1[:, :n], 0.044715, 1.0, ALU.mult, ALU.add)
                    P.tt(t1[:, :n], t1[:, :n], pp[:, :n], ALU.mult)
                    P.act(t2[:, :n], t1[:, :n], AF.Sigmoid, scale=1.5957691216)
                    P.tt(G[:, ct, :n], t2[:, :n], pp[:, :n], ALU.mult)
                else:
                    P.copy(X[:, ct - 8, :n], pp[:, :n], eng="scalar")
            P.dma(GTv[:, :, s0:s0 + n], G[:, :, :n])
            P.dma(XTv[:, :, s0:s0 + n], X[:, :, :n])
    P.barrier()
    with ExitStack() as st:
        def sb(name, shape, dt=F32):
            K.uid = getattr(K, "uid", 0) + 1
            return st.enter_context(nc.sbuf_tensor("%s_%d" % (name, K.uid), shape, dt))
        XP = sb("XP", [128, NT + 8])
        CO = 1; LO = 260
        cw = sb("cw", [128, 8, 4]); cb_ = sb("cb", [128, 8]); gb = sb("gb", [128, 2, 2, 8])
        lm = sb("lm", [128, 2, 8]); nsp8 = sb("nsp8", [128, 2, 8])
        gw = sb("gw", [128, 2, 2, 128], BF16)
        xc = sb("xc", [128, 512]); xcb = sb("xcb", [128, 512], BF16)
        rr = sb("rr", [128, 512]); ii = sb("ii", [128, 512]); aa = sb("aa", [128, 512]); bb = sb("bb", [128, 512])
        hh = sb("hh", [128, 512]); hf = sb("hf", [128, 512]); gg = sb("gg", [128, 512])
        zb = sb("zb", [128, 512], BF16); carry = sb("carry", [128, 1])
        P.dma(cw[:], conv_w); P.dma(cb_[:], conv_b); P.dma(gb[:], gate_b); P.dma(lm[:], lam)
        P.act(nsp8[:], lm[:], AF.Exp, scale=-1.0)
        P.act(nsp8[:], nsp8[:], AF.Ln, bias=1.0)
        P.ts(nsp8[:], nsp8[:], -8.0, None, ALU.mult)
        P.memset(XP[:], 0.0)
        for c in range(8):
            P.dma(XP[:, CO:CO + NCTX], XT[c * 128:(c + 1) * 128, 0:NCTX])
            P.dma(XP[:, LO:LO + SEQ], XT[c * 128:(c + 1) * 128, NCTX:NT])
            for d in range(2):
                for g in range(2):
                    P.dma(gw[:, d, g, :], gate_w[d, g, c], eng="gpsimd")
            for d in range(2):
                order = BLOCKS if d == 0 else ([BLOCKS[0]] + BLOCKS[:0:-1])
                P.memset(carry[:], 0.0)
                for (s0, n, isctx) in order:
                    base = (CO + s0) if isctx else (LO + s0 - NCTX)
                    P.ts(xc[:, :n], XP[:, base - 1:base - 1 + n], cw[:, c, 0:1], cb_[:, c:c + 1], ALU.mult, ALU.add)
                    for jj in range(1, 4):
                        P.stt(xc[:, :n], XP[:, base - 1 + jj:base - 1 + jj + n], cw[:, c, jj:jj + 1], xc[:, :n],
                              ALU.mult, ALU.add)
                    P.copy(xcb[:, :n], xc[:, :n], eng="gpsimd")
                    P.mm(ps[6][:, :n], gw[:, d, 0, :], xcb[:, :n])
                    P.mm(ps[7][:, :n], gw[:, d, 1, :], xcb[:, :n])
                    P.act(rr[:, :n], ps[6][:, :n], AF.Sigmoid, bias=gb[:, d, 0, c:c + 1])
                    P.act(ii[:, :n], ps[7][:, :n], AF.Sigmoid, bias=gb[:, d, 1, c:c + 1])
                    P.act(aa[:, :n], rr[:, :n], AF.Exp, scale=nsp8[:, d, c:c + 1])
                    P.tt(bb[:, :n], aa[:, :n], aa[:, :n], ALU.mult)
                    P.ts(bb[:, :n], bb[:, :n], -1.0, 1.0, ALU.mult, ALU.add)
                    P.ts(bb[:, :n], bb[:, :n], 0.0, None, ALU.max)
                    P.act(bb[:, :n], bb[:, :n], AF.Sqrt)
                    P.tt(ii[:, :n], ii[:, :n], xc[:, :n], ALU.mult, eng="gpsimd")
                    P.tt(bb[:, :n], bb[:, :n], ii[:, :n], ALU.mult)
                    if d == 0:
                        P.scan(hh[:, :n], aa[:, :n], bb[:, :n], carry[:, 0:1], ALU.mult, ALU.add)
                        P.copy(carry[:, 0:1], hh[:, n - 1:n], eng="scalar")
                        P.dma(HF[c * 128:(c + 1) * 128, s0:s0 + n], hh[:, :n])
                    else:
                        P.scan(hh[:, n - 1::-1], aa[:, n - 1::-1], bb[:, n - 1::-1], carry[:, 0:1], ALU.mult, ALU.add)
                        P.copy(carry[:, 0:1], hh[:, 0:1], eng="scalar")
                        P.dma(hf[:, :n], HF[c * 128:(c + 1) * 128, s0:s0 + n])
                        P.dma(gg[:, :n], GT[c * 128:(c + 1) * 128, s0:s0 + n])
                        P.tt(hh[:, :n], hh[:, :n], hf[:, :n], ALU.add)
                        P.tt(zb[:, :n], hh[:, :n], gg[:, :n], ALU.mult)
                        P.dma(ZT[c * 128:(c + 1) * 128, s0:s0 + n], zb[:, :n])
    P.barrier()
    return D


def declare_mixer_inputs(K, l):
    din = K.din
    kind = l % 4
    if kind == 3:
        K.hgrn_w_in = din("hgrn_w_in", [1, D, 5 * D]); K.hgrn_b_f = din("hgrn_b_f", [1, 128, 2, 8])
        K.hgrn_lb = din("hgrn_lb", [128, 8, 4]); K.hgrn_norm_g = din("hgrn_norm_g", [1, 128, 1])
        K.hg_masks = din("hg_masks", [128, 2, 128])
        if not hasattr(K, "w_out"):
            K.w_out = {}
        K.w_out[l] = din("hgrn_w_out", [1, D, D])[0]
    if kind == 1:
        K.rwkv_w_in = din("rwkv_w_in", [1, 3, D, D])
        K.rwkv_w_l1 = din("rwkv_w_l1", [1, 2, D, 64]); K.rwkv_w_l2 = din("rwkv_w_l2", [1, 2, 64, D])
        K.rwkv_a_l1 = din("rwkv_a_l1", [1, 2, D, 64]); K.rwkv_a_l2 = din("rwkv_a_l2", [1, 2, 64, D])
        K.rwkv_g_l1 = din("rwkv_g_l1", [1, D, 128]); K.rwkv_g_l2 = din("rwkv_g_l2", [1, 128, D])
        K.rwkv_vec = din("rwkv_vec", [1, 128, 16, 8]); K.rw_consts = din("rw_consts", [128, 5, 128])
        if not hasattr(K, "w_out"):
            K.w_out = {}
        K.w_out[l] = din("rwkv_w_out", [1, D, D])[0]
    if kind == 2:
        K.ret_w_in = din("ret_w_in", [1, D, 6 * D]); K.ret_decay = din("ret_decay", [1, 8])
        K.ret_gn_g = din("ret_gn_g", [1, 128, 16]); K.ret_gn_b = din("ret_gn_b", [1, 128, 16])
        K.ret_consts = din("ret_consts", [128, 5, 128]); K.ret_poscol = din("ret_poscol", [128, 2])
        K.rope_cos = din("rope_cos", [128, SEQ]); K.rope_sin = din("rope_sin", [128, SEQ])
        if not hasattr(K, "w_out"):
            K.w_out = {}
        K.w_out[l] = din("ret_w_out", [1, 2 * D, D])[0]

_CACHE = {}


def _pk(v):
    return np.ascontiguousarray(np.asarray(v, np.float32).reshape(-1, 128).T)


def host_inputs(inp, names):
    f32 = np.float32
    A = lambda v: np.ascontiguousarray(np.asarray(v, f32))
    oh4 = np.zeros((4, 4, 128), f32)
    for j in range(4):
        oh4[j, j, :] = 1.0
    d = {"ident": np.eye(128, dtype=f32), "oh4": oh4}
    if "x" in names:
        d["x"] = A(inp["x"][0]); d["ctx"] = A(inp["ctx"][0])
        d["cS"] = A(np.stack([_pk(inp["c"][0]), _pk(inp["c_ctx"])], axis=-1))
        d["ada_w"] = A(inp["ada_w"])
        d["ada_b"] = A(np.asarray(inp["ada_b"], f32).reshape(DEPTH, 48, 128).transpose(2, 0, 1))
        d["post_ln_g"] = A(inp["post_ln_g"]); d["post_ln_b"] = A(inp["post_ln_b"])
        d["moe_router"] = A(inp["moe_router"]); d["moe_bias"] = A(inp["moe_bias"])
    for l in range(DEPTH):
        if "moe_w_gu%d" % l in names:
            ne = MOE_EXPERTS_DECL or NEXP
            d["moe_w_gu%d" % l] = A(inp["moe_w_gu"][l][:ne]); d["moe_w_down%d" % l] = A(inp["moe_w_down"][l][:ne])
            d["moe_sh_gu%d" % l] = A(inp["moe_sh_gu"][l]); d["moe_sh_down%d" % l] = A(inp["moe_sh_down"][l])
    if "lru_w_in" in names:
        g_in = np.asarray(inp["lru_gate_w"], f32)
        gw = np.zeros((1, 2, 2, 8, 128, 128), f32)
        for dd in range(2):
            for g in range(2):
                for t in range(8):
                    gw[0, dd, g, t, 0:64, 0:64] = g_in[0, dd, g, 2 * t]
                    gw[0, dd, g, t, 64:128, 64:128] = g_in[0, dd, g, 2 * t + 1]
        d["lru_w_in"] = A(inp["lru_w_in"]); d["lru_w_out"] = A(inp["lru_w_out"])
        d["lru_conv_w"] = A(np.asarray(inp["lru_conv_w"], f32)[0].T.reshape(8, 128, 4).transpose(1, 0, 2)[None])
        d["lru_conv_b"] = A(_pk(inp["lru_conv_b"][0])[None])
        d["lru_gate_w"] = gw
        d["lru_gate_b"] = A(np.asarray(inp["lru_gate_b"], f32)[0].reshape(2, 2, 8, 128).transpose(3, 0, 1, 2)[None])
        d["lru_lam"] = A(np.asarray(inp["lru_lambda"], f32)[0].reshape(2, 8, 128).transpose(2, 0, 1)[None])
    if "hgrn_w_in" in names:
        d["hgrn_w_in"] = A(inp["hgrn_w_in"]); d["hgrn_w_out"] = A(inp["hgrn_w_out"])
        d["hgrn_b_f"] = A(np.asarray(inp["hgrn_b_f"], f32).reshape(1, 2, 8, 128).transpose(0, 3, 1, 2))
        d["hgrn_lb"] = A(np.asarray(inp["hgrn_lb"], f32).reshape(DEPTH, 8, 128).transpose(2, 1, 0))
        d["hgrn_norm_g"] = A(np.asarray(inp["hgrn_norm_g"], f32).reshape(1, 128, 1))
        m = np.zeros((128, 2, 128), f32)
        for s_ in range(128):
            for t_ in range(128):
                if s_ // 64 == t_ // 64:
                    m[s_, 0, t_] = 1.0 if s_ <= t_ else 0.0
                    m[s_, 1, t_] = 1.0 if s_ >= t_ else 0.0
        d["hg_masks"] = m
    if "ret_w_in" in names:
        d["ret_w_in"] = A(inp["ret_w_in"]); d["ret_w_out"] = A(inp["ret_w_out"])
        d["ret_decay"] = A(np.asarray(inp["ret_decay"], f32).reshape(1, 8))
        d["ret_gn_g"] = A(np.asarray(inp["ret_gn_g"], f32).reshape(1, 16, 128).transpose(0, 2, 1))
        d["ret_gn_b"] = A(np.asarray(inp["ret_gn_b"], f32).reshape(1, 16, 128).transpose(0, 2, 1))
        ss, tt = np.meshgrid(np.arange(128), np.arange(128), indexing="ij")
        cst = np.zeros((128, 5, 128), f32)
        cst[:, 0, :] = (ss <= tt); cst[:, 1, :] = (ss >= tt); cst[:, 2, :] = np.abs(tt - ss)
        cst[:, 3, :] = tt + 1; cst[:, 4, :] = 128 - tt
        d["ret_consts"] = cst
        d["ret_poscol"] = A(np.stack([127 - np.arange(128), np.arange(128)], axis=1))
        rows_ = SEQ // 64
        pos_row = np.repeat(np.arange(rows_, dtype=f32), 64); pos_col = np.tile(np.arange(64, dtype=f32), rows_)
        freqs = (f32(10000.0) ** (-np.arange(64, dtype=f32) / f32(64))).astype(f32)
        ang = np.concatenate([pos_row[:, None] * freqs, pos_col[:, None] * freqs], axis=-1).astype(f32)
        d["rope_cos"] = A(np.cos(ang).T); d["rope_sin"] = A(np.sin(ang).T)
    if "rwkv_w_in" in names:
        for k_ in ("rwkv_w_in", "rwkv_w_l1", "rwkv_w_l2", "rwkv_a_l1", "rwkv_a_l2", "rwkv_g_l1", "rwkv_g_l2", "rwkv_w_out"):
            d[k_] = A(inp[k_])
        ka_ = np.asarray(inp["rwkv_k_a"], f32)[0]
        rows_ = [np.asarray(inp["rwkv_mu"], f32)[0][i] for i in range(6)]
        rows_ += [np.asarray(inp["rwkv_w0"], f32)[0][z_] for z_ in range(2)]
        rows_ += [np.asarray(inp["rwkv_a0"], f32)[0][z_] for z_ in range(2)]
        rows_ += [np.asarray(inp["rwkv_k_k"], f32)[0], ka_, np.asarray(inp["rwkv_r_k"], f32)[0].reshape(-1),
                  np.asarray(inp["rwkv_ln_g"], f32)[0], np.asarray(inp["rwkv_ln_b"], f32)[0], ka_]
        vec = np.stack([_pk(r_) for r_ in rows_], axis=1)
        d["rwkv_vec"] = A(vec[None])
        tt_, ii_ = np.meshgrid(np.arange(128), np.arange(128), indexing="ij")
        same = (tt_ // 64 == ii_ // 64)
        cst = np.zeros((128, 5, 128), f32)
        cst[:, 0, :] = same & (ii_ < tt_); cst[:, 1, :] = same & (ii_ > tt_)
        cst[:, 2, :] = same & (tt_ <= ii_); cst[:, 3, :] = same & (tt_ >= ii_); cst[:, 4, :] = same
        d["rw_consts"] = cst
    return {k: d[k] for k in names if k in d}


def kernel(_n_layers=DEPTH, _stop_after=None, **inp):
    key = (_n_layers, _stop_after)
    if key not in _CACHE:
        _CACHE[key] = build_program(_n_layers, _stop_after)
    nc, K = _CACHE[key]
    real = host_inputs(inp, K.in_names)
    ncores = N_CORES
    maps = [real] + [{k: np.zeros_like(v) for k, v in real.items()} for _ in range(ncores - 1)]
    res = run_bass_kernel_spmd(nc, maps, core_ids=list(range(ncores)))
    return np.asarray(res.results[0]["out"], dtype=np.float32)[None]


def mixer_hgrn(K, l):
    nc, P, ps = K.nc, K.P, K.ps
    j = l // 4
    w_in = K.hgrn_w_in[j]; b_f = K.hgrn_b_f[j]; norm_g = K.hgrn_norm_g[j]
    OF = K.scrA
    C = 64
    with ExitStack() as st:
        def sb(name, shape, dt=F32):
            K.uid = getattr(K, "uid", 0) + 1
            return st.enter_context(nc.sbuf_tensor("%s_%d" % (name, K.uid), shape, dt))
        win = sb("hwin", [128, 8, 5 * D], BF16)
        P.dma(win[:], w_in.rearrange("(k p) n -> p k n", p=128), eng="gpsimd")
        bf = sb("hbf", [128, 2, 8]); lbr = sb("hlbr", [128, 8, 4]); lb = sb("hlb", [128, 8]); oml = sb("homl", [128, 8])
        ssum = sb("hss", [128, 8]); ng = sb("hng", [128, 1])
        P.dma(bf[:], b_f); P.dma(lbr[:], K.hgrn_lb); P.dma(ng[:], norm_g)
        P.act(lbr[:], lbr[:], AF.Exp)
        P.reduce(ssum[:], lbr[:], ALU.add)
        P.reduce(lb[:], lbr[:, :, 1:l + 1], ALU.add)
        P._add("vector", lambda e, a=ssum[:]: e.reciprocal(a, a), [ssum[:]], [ssum[:]])
        P.tt(lb[:], lb[:], ssum[:], ALU.mult)
        P.ts(oml[:], lb[:], -1.0, 1.0, ALU.mult, ALU.add)
        masks = sb("hmask", [128, 2, 128]); P.dma(masks[:], K.hg_masks)
        nst = sb("hnst", [128, 2, 512])
        P.memset(nst[:], 1.0)
        P.memset(nst[:, 0, 0:512:C], 0.0)
        P.memset(nst[:, 1, C - 1:512:C], 0.0)
        ones = sb("hones", [128, 128]); P.memset(ones[:], 1.0)
        S = sb("hS", [128, 8, 128]); Sb = sb("hSb", [128, 8, 128], BF16)
        u = [sb("hu%d" % i, [128, 8, 512], BF16) for i in range(2)]
        qh = sb("hq", [128, 512]); fh = sb("hf", [128, 512]); lf = sb("hlf", [128, 512]); kh = sb("hk", [128, 512])
        vT = sb("hvT", [128, 512]); B = sb("hB", [128, 512]); e1 = sb("he1", [128, 512])
        qt = sb("hqt", [128, 512], BF16); kt = sb("hkt", [128, 512], BF16); khT = sb("hkhT", [128, 512])
        V = sb("hV", [128, 4, 128], BF16); Kh = sb("hKh", [128, 4, 128], BF16)
        eBl = sb("heBl", [128, 8]); AmT = sb("hAmT", [128, 128], BF16); tmpI = sb("htI", [128, 128])
        oT = sb("hoT", [128, 512]); of = sb("hof", [128, 512]); gT = sb("hgT", [128, 512])
        rs = sb("hrs", [128, 512]); zb = sb("hzb", [128, 512], BF16)
        UTv = K.UT.rearrange("(k p) t -> p k t", p=128)

        def proj(dst_ps, col0, uu, n):
            for k in range(8):
                P.mm(dst_ps[:, :n], win[:, k, col0:col0 + 128], uu[:, k, :n], start=(k == 0), stop=(k == 7))

        it = 0
        for d in range(2):
            order = BLOCKS if d == 0 else ([BLOCKS[0]] + BLOCKS[:0:-1])
            P.memset(S[:], 0.0); P.memset(Sb[:], 0.0)
            for (s0, n, isctx) in order:
                uu = u[it % 2]; it += 1
                P.dma(uu[:, :, :n], UTv[:, :, s0:s0 + n])
                nch = n // C
                for h in range(8):
                    proj(ps[0], h * 128, uu, n)
                    P.act(qh[:, :n], ps[0][:, :n], AF.Silu)
                    proj(ps[1], (1 + d) * D + h * 128, uu, n)
                    P.act(fh[:, :n], ps[1][:, :n], AF.Sigmoid, bias=bf[:, d, h:h + 1])
                    P.ts(fh[:, :n], fh[:, :n], oml[:, h:h + 1], lb[:, h:h + 1], ALU.mult, ALU.add)
                    P.act(lf[:, :n], fh[:, :n], AF.Ln)
                    P.ts(kh[:, :n], fh[:, :n], -1.0, 1.0, ALU.mult, ALU.add)
                    proj(ps[0], 3 * D + h * 128, uu, n)
                    P.copy(vT[:, :n], ps[0][:, :n], eng="scalar")
                    for pi in range(n // 128):
                        P.transpose(ps[2][:, pi * 128:(pi + 1) * 128], vT[:, pi * 128:(pi + 1) * 128], K.ident[:])
                    P.copy(V[:, :n // 128, :].rearrange("p a b -> p (a b)"), ps[2][:, :n], eng="scalar")
                    if d == 0:
                        P.scan(B[:, :n], nst[:, 0, :n], lf[:, :n], 0.0, ALU.mult, ALU.add)
                    else:
                        P.scan(B[:, n - 1::-1], nst[:, 1, n - 1::-1], lf[:, n - 1::-1], 0.0, ALU.mult, ALU.add)
                    P.act(e1[:, :n], B[:, :n], AF.Exp)
                    P.tt(qt[:, :n], qh[:, :n], e1[:, :n], ALU.mult)
                    P.act(e1[:, :n], B[:, :n], AF.Exp, scale=-1.0)
                    P.tt(kt[:, :n], kh[:, :n], e1[:, :n], ALU.mult)
                    Bv = B[:, :n].rearrange("p (c t) -> p c t", t=C)
                    last = C - 1 if d == 0 else 0
                    Bl = Bv[:, :, last:last + 1]
                    P.act(eBl[:, :nch], Bv[:, :, last], AF.Exp)
                    P.tt(e1[:, :n].rearrange("p (c t) -> p c t", t=C), Bv, Bl.to_broadcast([128, nch, C]), ALU.subtract)
                    P.act(e1[:, :n], e1[:, :n], AF.Exp, scale=-1.0)
                    P.tt(khT[:, :n], kh[:, :n], e1[:, :n], ALU.mult)
                    for pi in range(n // 128):
                        P.transpose(ps[2][:, pi * 128:(pi + 1) * 128], khT[:, pi * 128:(pi + 1) * 128], K.ident[:])
                    P.copy(Kh[:, :n // 128, :].rearrange("p a b -> p (a b)"), ps[2][:, :n], eng="scalar")
                    prs = list(range(n // 128)) if d == 0 else list(range(n // 128 - 1, -1, -1))
                    for pi in prs:
                        c128 = slice(pi * 128, (pi + 1) * 128)
                        P.mm(ps[3][:, 0:128], kt[:, c128], qt[:, c128])
                        P.tt(AmT[:], ps[3][:, 0:128], masks[:, d, :], ALU.mult)
                        P.mm(ps[4][:, 0:128], V[:, pi, :], AmT[:])
                        for c in ([0, 1] if d == 0 else [1, 0]):
                            cs = slice(pi * 128 + c * C, pi * 128 + (c + 1) * C)
                            rows = slice(c * C, (c + 1) * C)
                            P.mm(ps[5][:, c * C:(c + 1) * C], Sb[:, h, :], qt[:, cs])
                            P.mm(ps[6][:, 0:128], Kh[rows, pi, :], V[rows, pi, :])
                            ch = pi * 2 + c
                            P.stt(S[:, h, :], S[:, h, :], eBl[:, ch:ch + 1], ps[6][:, 0:128], ALU.mult, ALU.add)
                            P.copy(Sb[:, h, :], S[:, h, :], eng="gpsimd")
                        P.copy(tmpI[:], ps[5][:, 0:128], eng="scalar")
                        P.tt(oT[:, c128], ps[4][:, 0:128], tmpI[:], ALU.add)
                    if d == 0:
                        P.dma(OF[h * 128:(h + 1) * 128, s0:s0 + n], oT[:, :n])
                    else:
                        P.dma(of[:, :n], OF[h * 128:(h + 1) * 128, s0:s0 + n])
                        P.tt(oT[:, :n], oT[:, :n], of[:, :n], ALU.add)
                        P.act(rs[:, :n], oT[:, :n], AF.Square)
                        P.mm(ps[7][:, :n], ones[:], rs[:, :n])
                        P.ts(rs[:, :n], ps[7][:, :n], 1.0 / 128, LN_EPS, ALU.mult, ALU.add)
                        P.act(rs[:, :n], rs[:, :n], AF.Sqrt)
                        P._add("vector", lambda e, a=rs[:, :n]: e.reciprocal(a, a), [rs[:, :n]], [rs[:, :n]])
                        P.stt(oT[:, :n], oT[:, :n], ng[:, 0:1], rs[:, :n], ALU.mult, ALU.mult)
                        proj(ps[1], 4 * D + h * 128, uu, n)
                        P.act(gT[:, :n], ps[1][:, :n], AF.Silu)
                        P.tt(zb[:, :n], oT[:, :n], gT[:, :n], ALU.mult)
                        P.dma(K.ZT[h * 128:(h + 1) * 128, s0:s0 + n], zb[:, :n])
    P.barrier()
    return D


def mixer_ret(K, l):
    nc, P, ps = K.nc, K.P, K.ps
    j = l // 4
    w_in = K.ret_w_in[j]
    OF = K.scrA
    C = 128
    with ExitStack() as st:
        def sb(name, shape, dt=F32):
            K.uid = getattr(K, "uid", 0) + 1
            return st.enter_context(nc.sbuf_tensor("%s_%d" % (name, K.uid), shape, dt))
        win = sb("rwin", [128, 8, 6 * D], BF16)
        P.dma(win[:], w_in.rearrange("(k p) n -> p k n", p=128), eng="gpsimd")
        cst = sb("rcst", [128, 5, 128]); P.dma(cst[:], K.ret_consts)
        pcol = sb("rpcol", [128, 2]); P.dma(pcol[:], K.ret_poscol)
        lg = sb("rlg", [128, 8]); P.dma(lg[:], _bcast_row(K.ret_decay[j:j + 1, :], 128, 8))
        gng = sb("rgng", [128, 16]); gnb = sb("rgnb", [128, 16])
        P.dma(gng[:], K.ret_gn_g[j]); P.dma(gnb[:], K.ret_gn_b[j])
        P.act(lg[:], lg[:], AF.Sigmoid)
        P.act(lg[:], lg[:], AF.Ln)
        Dm = sb("rDm", [128, 8, 128]); QD = sb("rQD", [128, 8, 128]); kdc = sb("rkdc", [128, 8]); g128 = sb("rg128", [128, 8])
        for dh in range(8):
            d = dh // 4
            P.act(Dm[:, dh, :], cst[:, 2, :], AF.Exp, scale=lg[:, dh:dh + 1])
            P.tt(Dm[:, dh, :], Dm[:, dh, :], cst[:, d, :], ALU.mult)
            P.act(QD[:, dh, :], cst[:, 3 + d, :], AF.Exp, scale=lg[:, dh:dh + 1])
            P.act(kdc[:, dh:dh + 1], pcol[:, d:d + 1], AF.Exp, scale=lg[:, dh:dh + 1])
        P.act(g128[:], lg[:], AF.Exp, scale=float(C))
        ones = sb("rones", [128, 128]); P.memset(ones[:], 1.0)
        S = sb("rS", [128, 4, 2, 512]); Sb = sb("rSb", [128, 4, 2, 512], BF16)
        u = [sb("ru0", [128, 8, 512], BF16)] * 2
        cs = sb("rcos", [128, 512]); sn = sb("rsin", [128, 512])
        qT = sb("rqT", [128, 2, 512]); kT = sb("rkT", [128, 2, 512]); r1 = sb("rr1", [128, 512]); r2 = sb("rr2", [128, 512])
        qR = sb("rqR", [128, 2, 512]); kR = sb("rkR", [128, 2, 512])
        qb = sb("rqb", [128, 2, 512], BF16); kb = sb("rkb", [128, 2, 512], BF16); qd = sb("rqd", [128, 2, 512], BF16)
        vT = sb("rvT", [128, 512])
        V = sb("rV", [128, 4, 512], BF16); Kh = sb("rKh", [128, 4, 256], BF16)
        AmT = sb("rAmT", [128, 128], BF16)
        oT = sb("roT", [128, 4, 512]); sq = sb("rsq", [128, 4, 512]); of = sq
        mean = sb("rmean", [128, 512]); rstd = sb("rrstd", [128, 512]); gT = sb("rgT", [128, 512]); zb = sb("rzb", [128, 512], BF16)
        UTv = K.UT.rearrange("(k p) t -> p k t", p=128)

        def proj(dst_ps, col0, uu, n):
            for k in range(8):
                P.mm(dst_ps[:, :n], win[:, k, col0:col0 + 128], uu[:, k, :n], start=(k == 0), stop=(k == 7))

        it = 0
        for d in range(2):
            order = BLOCKS if d == 0 else ([BLOCKS[0]] + BLOCKS[:0:-1])
            P.memset(S[:], 0.0); P.memset(Sb[:], 0.0)
            for (s0, n, isctx) in order:
                uu = u[it % 2]; it += 1
                P.dma(uu[:, :, :n], UTv[:, :, s0:s0 + n])
                nch = n // C
                if not isctx:
                    P.dma(cs[:, :n], K.rope_cos[:, s0 - NCTX:s0 - NCTX + n])
                    P.dma(sn[:, :n], K.rope_sin[:, s0 - NCTX:s0 - NCTX + n])
                for h in range(4):
                    dh = d * 4 + h
                    for kt in range(2):
                        proj(ps[kt], h * 256 + kt * 128, uu, n)
                        P.copy(qT[:, kt, :n], ps[kt][:, :n], eng="scalar")
                    for kt in range(2):
                        proj(ps[kt], D + h * 256 + kt * 128, uu, n)
                        P.act(kT[:, kt, :n], ps[kt][:, :n], AF.Copy, scale=1.0 / 16.0)
                    if isctx:
                        zq, zk = qT, kT
                    else:
                        for (src, dst) in ((qT, qR), (kT, kR)):
                            P.tt(r1[:, :n], src[:, 0, :n], cs[:, :n], ALU.mult)
                            P.tt(r2[:, :n], src[:, 1, :n], sn[:, :n], ALU.mult, eng="gpsimd")
                            P.tt(dst[:, 0, :n], r1[:, :n], r2[:, :n], ALU.subtract)
                            P.tt(r1[:, :n], src[:, 1, :n], cs[:, :n], ALU.mult)
                            P.tt(r2[:, :n], src[:, 0, :n], sn[:, :n], ALU.mult, eng="gpsimd")
                            P.tt(dst[:, 1, :n], r1[:, :n], r2[:, :n], ALU.add)
                        zq, zk = qR, kR
                    P.copy(qb[:, :, :n], zq[:, :, :n], eng="gpsimd")
                    P.copy(kb[:, :, :n], zk[:, :, :n], eng="gpsimd")
                    for kt in range(2):
                        P.tt(qd[:, kt, :n].rearrange("p (c t) -> p c t", t=C), zq[:, kt, :n].rearrange("p (c t) -> p c t", t=C),
                             QD[:, dh, :].unsqueeze(1).to_broadcast([128, nch, C]), ALU.mult)
                    for vt in range(4):
                        proj(ps[vt % 2], 2 * D + h * 512 + vt * 128, uu, n)
                        P.copy(vT[:, :n], ps[vt % 2][:, :n], eng="scalar")
                        for ci in range(nch):
                            P.transpose(ps[2][:, ci * 128:(ci + 1) * 128], vT[:, ci * 128:(ci + 1) * 128], K.ident[:])
                        P.copy(V[:, :nch, vt * 128:(vt + 1) * 128], ps[2][:, :n].rearrange("p (c t) -> p c t", t=128), eng="scalar")
                    for kt in range(2):
                        for ci in range(nch):
                            P.transpose(ps[2][:, ci * 128:(ci + 1) * 128], zk[:, kt, ci * 128:(ci + 1) * 128], K.ident[:])
                        P.ts(Kh[:, :nch, kt * 128:(kt + 1) * 128], ps[2][:, :n].rearrange("p (c t) -> p c t", t=128),
                             kdc[:, dh:dh + 1], None, ALU.mult)
                    chs = list(range(nch)) if d == 0 else list(range(nch - 1, -1, -1))
                    for ci in chs:
                        cc = slice(ci * C, (ci + 1) * C)
                        for kt in range(2):
                            P.mm(ps[3][:, 0:128], kb[:, kt, cc], qb[:, kt, cc], start=(kt == 0), stop=(kt == 1))
                        P.tt(AmT[:], ps[3][:, 0:128], Dm[:, dh, :], ALU.mult)
                        for vt in range(4):
                            po = ps[4][:, vt * 128:(vt + 1) * 128]
                            P.mm(po, V[:, ci, vt * 128:(vt + 1) * 128], AmT[:], start=True, stop=False)
                            P.mm(po, Sb[:, h, 0, vt * 128:(vt + 1) * 128], qd[:, 0, cc], start=False, stop=False)
                            P.mm(po, Sb[:, h, 1, vt * 128:(vt + 1) * 128], qd[:, 1, cc], start=False, stop=True)
                        P.copy(oT[:, :, cc], ps[4][:, :].rearrange("p (v t) -> p v t", t=128), eng="scalar")
                        for kt in range(2):
                            P.mm(ps[5 + kt][:, :], Kh[:, ci, kt * 128:(kt + 1) * 128], V[:, ci, :])
                            P.stt(S[:, h, kt, :], S[:, h, kt, :], g128[:, dh:dh + 1], ps[5 + kt][:, :], ALU.mult, ALU.add)
                            P.copy(Sb[:, h, kt, :], S[:, h, kt, :], eng="gpsimd")
                    rows = lambda vt: slice(h * 512 + vt * 128, h * 512 + (vt + 1) * 128)
                    if d == 0:
                        for vt in range(4):
                            P.dma(OF[rows(vt), s0:s0 + n], oT[:, vt, :n])
                    else:
                        for vt in range(4):
                            P.dma(of[:, vt, :n], OF[rows(vt), s0:s0 + n])
                        P.tt(oT[:, :, :n], oT[:, :, :n], of[:, :, :n], ALU.add)
                        P.act(sq[:, :, :n], oT[:, :, :n], AF.Square)
                        for vt in range(4):
                            P.mm(ps[7][:, :n], ones[:], oT[:, vt, :n], start=(vt == 0), stop=(vt == 3))
                        for vt in range(4):
                            P.mm(ps[3][:, :n], ones[:], sq[:, vt, :n], start=(vt == 0), stop=(vt == 3))
                        P.act(mean[:, :n], ps[7][:, :n], AF.Copy, scale=1.0 / 512)
                        P.tt(rstd[:, :n], mean[:, :n], mean[:, :n], ALU.mult)
                        P.stt(rstd[:, :n], ps[3][:, :n], 1.0 / 512, rstd[:, :n], ALU.mult, ALU.subtract)
                        P.ts(rstd[:, :n], rstd[:, :n], LN_EPS, None, ALU.add)
                        P.act(rstd[:, :n], rstd[:, :n], AF.Sqrt)
                        P._add("vector", lambda e, a=rstd[:, :n]: e.reciprocal(a, a), [rstd[:, :n]], [rstd[:, :n]])
                        for vt in range(4):
                            gi = h * 4 + vt
                            P.tt(sq[:, vt, :n], oT[:, vt, :n], mean[:, :n], ALU.subtract)
                            P.tt(sq[:, vt, :n], sq[:, vt, :n], rstd[:, :n], ALU.mult)
                            P.ts(sq[:, vt, :n], sq[:, vt, :n], gng[:, gi:gi + 1], gnb[:, gi:gi + 1], ALU.mult, ALU.add)
                            proj(ps[vt % 2], 4 * D + h * 512 + vt * 128, uu, n)
                            P.act(gT[:, :n], ps[vt % 2][:, :n], AF.Silu)
                            P.tt(zb[:, :n], sq[:, vt, :n], gT[:, :n], ALU.mult)
                            P.dma(K.ZT[rows(vt), s0:s0 + n], zb[:, :n])
    P.barrier()
    return 2 * D


def mixer_rwkv(K, l):
    nc, P, ps = K.nc, K.P, K.ps
    j = l // 4
    OF = K.scrA; BN = K.scrB
    C = 64
    DS = 0.6065306597126334
    with ExitStack() as st:
        def sb(name, shape, dt=F32):
            K.uid = getattr(K, "uid", 0) + 1
            return st.enter_context(nc.sbuf_tensor("%s_%d" % (name, K.uid), shape, dt))
        W3 = sb("wW3", [128, 3, 8, D], BF16)
        for i in range(3):
            P.dma(W3[:, i], K.rwkv_w_in[j, i].rearrange("(k p) n -> p k n", p=128), eng="gpsimd")
        wl1 = sb("wwl1", [128, 2, 8, 64], BF16); al1 = sb("wal1", [128, 2, 8, 64], BF16); gl1 = sb("wgl1", [128, 8, 128], BF16)
        wl2 = sb("wwl2", [64, 2, D], BF16); al2 = sb("wal2", [64, 2, D], BF16); gl2 = sb("wgl2", [128, D], BF16)
        for z in range(2):
            P.dma(wl1[:, z], K.rwkv_w_l1[j, z].rearrange("(k p) n -> p k n", p=128), eng="gpsimd")
            P.dma(al1[:, z], K.rwkv_a_l1[j, z].rearrange("(k p) n -> p k n", p=128), eng="gpsimd")
            P.dma(wl2[:, z], K.rwkv_w_l2[j, z], eng="gpsimd")
            P.dma(al2[:, z], K.rwkv_a_l2[j, z], eng="gpsimd")
        P.dma(gl1[:], K.rwkv_g_l1[j].rearrange("(k p) n -> p k n", p=128), eng="gpsimd")
        P.dma(gl2[:], K.rwkv_g_l2[j], eng="gpsimd")
        vec = sb("wvec", [128, 16, 8]); P.dma(vec[:], K.rwkv_vec[j])
        omm = sb("womm", [128, 6, 8]); P.ts(omm[:], vec[:, 0:6, :], -1.0, 1.0, ALU.mult, ALU.add)
        P.ts(vec[:, 15, :], vec[:, 15, :], -1.0, 1.0, ALU.mult, ALU.add)
        cst = sb("wcst", [128, 5, 128]); P.dma(cst[:], K.rw_consts)
        bones = cst[:, 4, :]
        nst = sb("wnst", [128, 2, 512]); P.memset(nst[:], 1.0)
        P.memset(nst[:, 0, 0:512:C], 0.0); P.memset(nst[:, 1, C - 1:512:C], 0.0)
        ue = sb("wue", [128, 8, 514], BF16); xm = sb("wxm", [128, 4, 8, 512], BF16); tq = sb("wtq", [128, 512], BF16)
        hw = sb("whw", [64, 512], BF16); ha = sb("wha", [64, 512], BF16); hg = sb("whg", [128, 512], BF16)
        F = lambda nm: sb(nm, [128, 512])
        rT = F("wrT"); kT = F("wkT"); vT = F("wvT"); lw = F("wlw"); aT = F("waT"); kk = F("wkk"); ka = F("wka"); kt = F("wkt")
        B = F("wB"); e1 = F("we1"); e2 = F("we2"); t1 = F("wt1"); oT = F("woT"); of = F("wof"); bn = F("wbn"); gT = F("wgT")
        zb = sb("wzb", [128, 512], BF16)
        names = ["kap", "bt", "ktl", "rt", "Kh", "Bh", "Vb"]
        bd = {nm: sb("wbd" + nm, [128, 8, 128]) for nm in names}
        for nm in names:
            P.memset(bd[nm][:], 0.0)
        tok = {nm: sb("wtok" + nm, [128, 8, 128]) for nm in ("V", "Kh", "Bh")}
        eBl = sb("weBl", [128, 8])
        Sk = sb("wSk", [128, 8, 128])
        Nm = [sb("wN%d" % i, [128, 128]) for i in range(2)]; Nt = [sb("wNt%d" % i, [128, 128]) for i in range(2)]
        Tt = [sb("wTt%d" % i, [128, 128]) for i in range(2)]
        NkT = sb("wNkT", [128, 128]); MkT = sb("wMkT", [128, 128]); MbT = sb("wMbT", [128, 128])
        RH = sb("wRH", [128, 128]); nU = sb("wnU", [128, 128])
        UTv = K.UT.rearrange("(k p) t -> p k t", p=128)

        def projW(dst_ps, i, col0, n):
            for k in range(8):
                P.mm(dst_ps[:, :n], W3[:, i, k, col0:col0 + 128], xm[:, i, k, :n], start=(k == 0), stop=(k == 7))

        def bsum(dst_ps, src, n):
            P.mm(dst_ps[:, :n], bones, src, start=True, stop=True)

        for z in range(2):
            order = BLOCKS if z == 0 else ([BLOCKS[0]] + BLOCKS[:0:-1])
            P.memset(Sk[:], 0.0)
            for (s0, n, isctx) in order:
                nch = n // C
                seg0, seg1 = (0, NCTX) if isctx else (NCTX, NT)
                P.memset(ue[:, :, 0:1], 0.0); P.memset(ue[:, :, n + 1:n + 2], 0.0)
                P.dma(ue[:, :, 1:n + 1], UTv[:, :, s0:s0 + n])
                if s0 > seg0:
                    P.dma(ue[:, :, 0:1], UTv[:, :, s0 - 1:s0], allow_slow_non_contiguous=True)
                if s0 + n < seg1:
                    P.dma(ue[:, :, n + 1:n + 2], UTv[:, :, s0 + n:s0 + n + 1], allow_slow_non_contiguous=True)
                def mix(i, slot):
                    for k in range(8):
                        shv = ue[:, k, 0:n] if k < 4 else ue[:, k, 2:n + 2]
                        P.ts(tq[:, :n], ue[:, k, 1:n + 1], omm[:, i, k:k + 1], None, ALU.mult, eng="gpsimd")
                        P.stt(xm[:, slot, k, :n], shv, vec[:, i, k:k + 1], tq[:, :n], ALU.mult, ALU.add)
                for i in range(3):
                    mix(i, i)
                mix(3, 3)
                for k in range(8):
                    P.mm(ps[0][0:64, :n], wl1[:, z, k, :], xm[:, 3, k, :n], start=(k == 0), stop=(k == 7))
                P.act(hw[:, :n], ps[0][0:64, :n], AF.Tanh)
                mix(4, 3)
                for k in range(8):
                    P.mm(ps[1][0:64, :n], al1[:, z, k, :], xm[:, 3, k, :n], start=(k == 0), stop=(k == 7))
                P.copy(ha[:, :n], ps[1][0:64, :n], eng="scalar")
                if z == 1:
                    mix(5, 3)
                    for k in range(8):
                        P.mm(ps[0][:, :n], gl1[:, k, :], xm[:, 3, k, :n], start=(k == 0), stop=(k == 7))
                    P.act(hg[:, :n], ps[0][:, :n], AF.Sigmoid)
                for pt in range(8):
                    c0 = pt * 128
                    projW(ps[0], 0, c0, n); P.copy(rT[:, :n], ps[0][:, :n], eng="scalar")
                    projW(ps[1], 1, c0, n); P.copy(kT[:, :n], ps[1][:, :n], eng="scalar")
                    projW(ps[0], 2, c0, n); P.copy(vT[:, :n], ps[0][:, :n], eng="scalar")
                    P.mm(ps[1][:, :n], wl2[:, z, c0:c0 + 128], hw[:, :n])
                    P.act(lw[:, :n], ps[1][:, :n], AF.Sigmoid, bias=vec[:, 6 + z, pt:pt + 1])
                    P.ts(lw[:, :n], lw[:, :n], -DS, None, ALU.mult)
                    P.mm(ps[0][:, :n], al2[:, z, c0:c0 + 128], ha[:, :n])
                    P.act(aT[:, :n], ps[0][:, :n], AF.Sigmoid, bias=vec[:, 8 + z, pt:pt + 1])
                    P.ts(kk[:, :n], kT[:, :n], vec[:, 10, pt:pt + 1], None, ALU.mult)
                    P.tt(t1[:, :n], kk[:, :n], kk[:, :n], ALU.mult)
                    bsum(ps[2], t1[:, :n], n)
                    P.ts(t1[:, :n], ps[2][:, :n], 1e-12, None, ALU.add)
                    P.act(t1[:, :n], t1[:, :n], AF.Sqrt)
                    P._add("vector", lambda e, a=t1[:, :n]: e.reciprocal(a, a), [t1[:, :n]], [t1[:, :n]])
                    P.tt(kk[:, :n], kk[:, :n], t1[:, :n], ALU.mult)
                    P.tt(ka[:, :n], kk[:, :n], aT[:, :n], ALU.mult)
                    P.ts(t1[:, :n], aT[:, :n], vec[:, 11, pt:pt + 1], vec[:, 15, pt:pt + 1], ALU.mult, ALU.add)
                    P.tt(kt[:, :n], kT[:, :n], t1[:, :n], ALU.mult)
                    P.stt(t1[:, :n], rT[:, :n], vec[:, 12, pt:pt + 1], kt[:, :n], ALU.mult, ALU.mult)
                    bsum(ps[2], t1[:, :n], n)
                    P.tt(bn[:, :n], ps[2][:, :n], vT[:, :n], ALU.mult)
                    if z == 0:
                        P.scan(B[:, :n], nst[:, 0, :n], lw[:, :n], 0.0, ALU.mult, ALU.add)
                    else:
                        P.scan(B[:, n - 1::-1], nst[:, 1, n - 1::-1], lw[:, n - 1::-1], 0.0, ALU.mult, ALU.add)
                    cv = lambda ap, lo: ap[lo:lo + 64, :n].rearrange("p (c t) -> p c t", t=C)
                    def to_bd(nm, a_, b_):
                        for hh in range(2):
                            lo = hh * 64
                            P.tt(bd[nm][lo:lo + 64, :nch, lo:lo + 64], cv(a_, lo), cv(b_, lo), ALU.mult)
                    P.tt(e1[:, :n], B[:, :n], lw[:, :n], ALU.subtract)
                    P.act(e1[:, :n], e1[:, :n], AF.Exp)
                    to_bd("kap", kk, e1)
                    P.act(e1[:, :n], B[:, :n], AF.Exp)
                    to_bd("rt", rT, e1)
                    P.act(e1[:, :n], B[:, :n], AF.Exp, scale=-1.0)
                    to_bd("bt", ka, e1)
                    to_bd("ktl", kt, e1)
                    Bv = B[:, :n].rearrange("p (c t) -> p c t", t=C)
                    last = C - 1 if z == 0 else 0
                    P.act(eBl[:, :nch], Bv[:, :, last], AF.Exp)
                    P.tt(e2[:, :n].rearrange("p (c t) -> p c t", t=C), Bv, Bv[:, :, last:last + 1].to_broadcast([128, nch, C]), ALU.subtract)
                    P.act(e2[:, :n], e2[:, :n], AF.Exp, scale=-1.0)
                    to_bd("Kh", kt, e2)
                    to_bd("Bh", ka, e2)
                    for hh in range(2):
                        lo = hh * 64
                        P.copy(bd["Vb"][lo:lo + 64, :nch, lo:lo + 64], cv(vT, lo), eng="gpsimd")
                    for nm, src in (("V", "Vb"), ("Kh", "Kh"), ("Bh", "Bh")):
                        for g4 in range(0, nch, 4):
                            for c in range(g4, g4 + 4):
                                P.transpose(ps[3][:, (c - g4) * 128:(c - g4 + 1) * 128], bd[src][:, c, :], K.ident[:])
                            P.copy(tok[nm][:, g4:g4 + 4, :].rearrange("p c t -> p (c t)"), ps[3][:, :], eng="scalar")
                    sl, su, ui, li = (cst[:, i, :] for i in range(4))
                    m_strict_ti = sl if z == 0 else su
                    m_strict_it = su if z == 0 else sl
                    m_incl_it = ui if z == 0 else li
                    chs = list(range(nch)) if z == 0 else list(range(nch - 1, -1, -1))
                    for c in chs:
                        kap = bd["kap"][:, c, :]; bt = bd["bt"][:, c, :]; ktl = bd["ktl"][:, c, :]; rt_ = bd["rt"][:, c, :]
                        P.mm(ps[4][:, 0:128], kap, bt)
                        P.mm(ps[4][:, 128:256], bt, kap)
                        P.mm(ps[4][:, 256:384], ktl, kap)
                        P.mm(ps[4][:, 384:512], ktl, rt_)
                        P.mm(ps[5][:, 384:512], bt, rt_)
                        P.tt(Nm[0][:], ps[4][:, 0:128], m_strict_ti, ALU.mult)
                        P.tt(Nt[0][:], ps[4][:, 128:256], m_strict_it, ALU.mult)
                        P.tt(NkT[:], ps[4][:, 256:384], m_strict_it, ALU.mult)
                        P.tt(MkT[:], ps[4][:, 384:512], m_incl_it, ALU.mult)
                        P.tt(MbT[:], ps[5][:, 384:512], m_incl_it, ALU.mult)
                        P.tt(Tt[0][:], K.ident[:], Nt[0][:], ALU.subtract)
                        cur = 0
                        for lvl in range(5):
                            X, Xt, T_ = Nm[cur], Nt[cur], Tt[cur]
                            X2, Xt2, T2 = Nm[1 - cur], Nt[1 - cur], Tt[1 - cur]
                            P.mm(ps[5][:, 0:128], Xt[:], X[:])
                            if lvl < 4:
                                P.mm(ps[5][:, 128:256], X[:], Xt[:])
                            P.copy(X2[:], ps[5][:, 0:128], eng="scalar")
                            if lvl < 4:
                                P.copy(Xt2[:], ps[5][:, 128:256], eng="scalar")
                            P.mm(ps[5][:, 256:384], X2[:], T_[:])
                            P.tt(T2[:], ps[5][:, 256:384], T_[:], ALU.add)
                            cur = 1 - cur
                        TT = Tt[cur]
                        P.mm(ps[7][:, 0:128], kap, Sk[:, pt, :], start=True, stop=False)
                        P.mm(ps[7][:, 0:128], NkT[:], tok["V"][:, c, :], start=False, stop=True)
                        P.copy(RH[:], ps[7][:, 0:128], eng="scalar")
                        P.mm(ps[7][:, 128:256], TT[:], RH[:])
                        P.act(nU[:], ps[7][:, 128:256], AF.Copy, scale=-1.0)
                        P.mm(ps[7][:, 256:384], Sk[:, pt, :], rt_, start=True, stop=False)
                        P.mm(ps[7][:, 256:384], tok["V"][:, c, :], MkT[:], start=False, stop=False)
                        P.mm(ps[7][:, 256:384], nU[:], MbT[:], start=False, stop=True)
                        for hh in range(2):
                            lo = hh * 64
                            P.copy(oT[lo:lo + 64, c * C:(c + 1) * C], ps[7][lo:lo + 64, 256 + lo:256 + lo + 64], eng="scalar")
                        P.mm(ps[7][:, 384:512], tok["Kh"][:, c, :], tok["V"][:, c, :], start=True, stop=False)
                        P.mm(ps[7][:, 384:512], tok["Bh"][:, c, :], nU[:], start=False, stop=True)
                        P.stt(Sk[:, pt, :], Sk[:, pt, :], eBl[:, c:c + 1], ps[7][:, 384:512], ALU.mult, ALU.add)
                    rows = slice(c0, c0 + 128)
                    if z == 0:
                        P.dma(OF[rows, s0:s0 + n], oT[:, :n])
                        P.dma(BN[rows, s0:s0 + n], bn[:, :n])
                    else:
                        P.dma(of[:, :n], OF[rows, s0:s0 + n])
                        P.tt(oT[:, :n], oT[:, :n], of[:, :n], ALU.add)
                        P.dma(of[:, :n], BN[rows, s0:s0 + n])
                        P.tt(bn[:, :n], bn[:, :n], of[:, :n], ALU.add)
                        bsum(ps[2], oT[:, :n], n)
                        P.act(e1[:, :n], ps[2][:, :n], AF.Copy, scale=1.0 / 64)
                        P.tt(oT[:, :n], oT[:, :n], e1[:, :n], ALU.subtract)
                        P.tt(t1[:, :n], oT[:, :n], oT[:, :n], ALU.mult)
                        bsum(ps[2], t1[:, :n], n)
                        P.ts(t1[:, :n], ps[2][:, :n], 1.0 / 64, 64e-5, ALU.mult, ALU.add)
                        P.act(t1[:, :n], t1[:, :n], AF.Sqrt)
                        P._add("vector", lambda e, a=t1[:, :n]: e.reciprocal(a, a), [t1[:, :n]], [t1[:, :n]])
                        P.tt(oT[:, :n], oT[:, :n], t1[:, :n], ALU.mult)
                        P.ts(oT[:, :n], oT[:, :n], vec[:, 13, pt:pt + 1], vec[:, 14, pt:pt + 1], ALU.mult, ALU.add)
                        P.tt(oT[:, :n], oT[:, :n], bn[:, :n], ALU.add)
                        P.mm(ps[0][:, :n], gl2[:, c0:c0 + 128], hg[:, :n])
                        P.tt(zb[:, :n], oT[:, :n], ps[0][:, :n], ALU.mult)
                        P.dma(K.ZT[rows, s0:s0 + n], zb[:, :n])
    P.barrier()
    return D
```

```python
import numpy as np
from contextlib import ExitStack
import concourse.bass as bass
import concourse.mybir as mybir
from concourse.bass_utils import run_bass_kernel_spmd

F32 = mybir.dt.float32
BF16 = mybir.dt.bfloat16
I32 = mybir.dt.int32
U32 = mybir.dt.uint32
AF = mybir.ActivationFunctionType
ALU = mybir.AluOpType
AX = mybir.AxisListType

NDSEM = 12


class Prog:
    ENGS = ["sync", "scalar", "vector", "gpsimd", "tensor"]

    def __init__(self, nc):
        self.nc = nc
        self.ops = []
        self.acc = {}
        self.untracked = set()

    def _region(self, ap):
        name = ap.tensor.name
        if name in self.untracked:
            return None
        pairs = ap.ap
        off = int(ap.offset)
        space = str(ap.space)
        if space == "PSUM":
            return (name, 0, 128, 0, 1 << 30)
        if space in ("SB", "PSUM"):
            pstride = pairs[0][0]
            if pstride == 0:
                pstride = 1 << 40
            p0 = off // pstride
            p1 = p0 + pairs[0][1]
            f0 = off % pstride
            ext = 1
            for st, cn in pairs[1:]:
                ext += abs(st) * (cn - 1)
            return (name, p0, p1, f0, f0 + ext)
        else:
            ext = 1
            for st, cn in pairs:
                ext += abs(st) * (cn - 1)
            return (name, 0, 1, off, off + ext)

    @staticmethod
    def _ovl(a, b):
        return a[1] < b[2] and b[1] < a[2] and a[3] < b[4] and b[3] < a[4]

    @staticmethod
    def _covers(a, b):
        return a[1] <= b[1] and a[2] >= b[2] and a[3] <= b[3] and a[4] >= b[4]

    def _add(self, eng, fn, reads, writes, dma=False):
        opid = len(self.ops)
        deps = set()
        rregs = [r for r in (self._region(a) for a in reads if a is not None) if r is not None]
        wregs = [r for r in (self._region(a) for a in writes if a is not None) if r is not None]
        for r in rregs:
            for rec in self.acc.get(r[0], ()):
                if rec[2] and self._ovl(r, rec[0]):
                    deps.add(rec[1])
        for w in wregs:
            for rec in self.acc.get(w[0], ()):
                if self._ovl(w, rec[0]):
                    deps.add(rec[1])
        for w in wregs:
            lst = self.acc.setdefault(w[0], [])
            lst[:] = [rec for rec in lst if not self._covers(w, rec[0])]
            lst.append((w, opid, True, eng, dma))
        for r in rregs:
            lst = self.acc.setdefault(r[0], [])
            lst[:] = [rec for rec in lst if not ((not rec[2]) and rec[3] == eng and not dma and not rec[4] and rec[0] == r)]
            lst.append((r, opid, False, eng, dma))
        deps |= getattr(self, "bar", set())
        self.ops.append(dict(eng=eng, fn=fn, deps=deps, dma=dma))
        return opid

    def dma(self, out, in_, eng="sync", **kw):
        return self._add(eng, lambda e: e.dma_start(out=out, in_=in_, **kw), [in_], [out], dma=True)

    def mm(self, out, lhsT, rhs, start=True, stop=True, **kw):
        return self._add("tensor", lambda e: e.matmul(out, lhsT, rhs, start=start, stop=stop, **kw),
                         [lhsT, rhs] + ([] if start else [out]), [out])

    def transpose(self, out, in_, ident):
        return self._add("tensor", lambda e: e.transpose(out, in_, ident), [in_, ident], [out])

    def act(self, out, in_, func, bias=None, scale=None, accum_out=None, eng="scalar"):
        kw = {}
        rd = [in_]
        if bias is not None:
            kw["bias"] = bias
            if not isinstance(bias, (int, float)):
                rd.append(bias)
        if scale is not None:
            kw["scale"] = scale
            if not isinstance(scale, (int, float)):
                rd.append(scale)
        wr = [out]
        if accum_out is not None:
            kw["accum_out"] = accum_out
            wr.append(accum_out)
        return self._add(eng, lambda e: e.activation(out, in_, func, **kw), rd, wr)

    def tt(self, out, in0, in1, op, eng="vector"):
        return self._add(eng, lambda e: e.tensor_tensor(out, in0, in1, op), [in0, in1], [out])

    def ts(self, out, in0, s1, s2, op0, op1=None, accum_out=None, eng="vector"):
        rd = [in0]
        if not isinstance(s1, (int, float)):
            rd.append(s1)
        if s2 is not None and not isinstance(s2, (int, float)):
            rd.append(s2)
        wr = [out]
        kw = {}
        if accum_out is not None:
            kw["accum_out"] = accum_out
            wr.append(accum_out)
        if op1 is None:
            return self._add(eng, lambda e: e.tensor_scalar(out, in0, s1, None, op0, **kw), rd, wr)
        return self._add(eng, lambda e: e.tensor_scalar(out, in0, s1, s2, op0, op1, **kw), rd, wr)

    def stt(self, out, in0, scalar, in1, op0, op1, eng="vector"):
        rd = [in0, in1]
        if not isinstance(scalar, (int, float)):
            rd.append(scalar)
        return self._add(eng, lambda e: e.scalar_tensor_tensor(out, in0, scalar, in1, op0, op1), rd, [out])

    def copy(self, out, in_, eng="vector"):
        if eng == "scalar":
            return self._add(eng, lambda e: e.copy(out, in_), [in_], [out])
        return self._add(eng, lambda e: e.tensor_copy(out, in_), [in_], [out])

    def memset(self, ap, val, eng="vector"):
        return self._add(eng, lambda e: e.memset(ap, val), [], [ap])

    def reduce(self, out, in_, op, axis=None, eng="vector"):
        axis = axis or AX.X
        return self._add(eng, lambda e: e.tensor_reduce(out, in_, axis, op), [in_], [out])

    def scan(self, out, d0, d1, initial, op0, op1, eng="vector"):
        rd = [d0, d1]
        if not isinstance(initial, (int, float)):
            rd.append(initial)
        return self._add(eng, lambda e: e.tensor_tensor_scan(out, d0, d1, initial, op0, op1), rd, [out])

    def barrier(self):
        last = {}
        for i, op in enumerate(self.ops):
            last[(op["eng"], op["dma"])] = i
        dm = {}
        for i, op in enumerate(self.ops):
            if op["dma"]:
                dm.setdefault(op["eng"], []).append(i)
        b = set(i for (e, isd), i in last.items() if not isd)
        for q, lst in dm.items():
            b.update(lst[-NDSEM:])
        self.bar = b
        self.acc = {}

    def generic(self, eng, fn, reads, writes):
        return self._add(eng, fn, reads, writes)

    def emit(self):
        nc = self.nc
        ops = self.ops
        with ExitStack() as st:
            csem = {e: st.enter_context(nc.semaphore("c_" + e)) for e in ["scalar", "vector", "gpsimd", "tensor"]}
            dsem = {q: [st.enter_context(nc.semaphore("d_%s%d" % (q, i))) for i in range(NDSEM)]
                    for q in ["sync", "gpsimd", "scalar"]}
            cnt = {e: 0 for e in csem}
            dcnt = {q: 0 for q in dsem}
            for op in ops:
                if op["dma"]:
                    q = op["eng"]
                    i = dcnt[q]
                    dcnt[q] += 1
                    op["sem"] = dsem[q][i % NDSEM]
                    op["val"] = 16 * (i // NDSEM + 1)
                    op["prev"] = 16 * (i // NDSEM)
                else:
                    e = op["eng"]
                    cnt[e] += 1
                    op["sem"] = csem[e]
                    op["val"] = cnt[e]
            self.stats = dict(cnt=dict(cnt), dcnt=dict(dcnt), nwait=0)

            def run(engname):
                def f(e):
                    waited = {}
                    for op in ops:
                        if op["eng"] != engname:
                            continue
                        needs = {}
                        for d in op["deps"]:
                            dop = ops[d]
                            if engname == "tensor" and dop["eng"] == "tensor" and not dop["dma"]:
                                continue
                            s = dop["sem"]
                            if needs.get(s, (None, 0))[1] < dop["val"]:
                                needs[s] = (s, dop["val"])
                        if op["dma"] and op["prev"] > 0:
                            s = op["sem"]
                            if needs.get(s, (None, 0))[1] < op["prev"]:
                                needs[s] = (s, op["prev"])
                        for s, v in needs.values():
                            if waited.get(s, 0) < v:
                                e.wait_ge(s, v)
                                waited[s] = v
                                self.stats["nwait"] += 1
                        ins = op["fn"](e)
                        ins.then_inc(op["sem"], 16 if op["dma"] else 1)
                    if engname == "sync":
                        for q in dsem:
                            n = dcnt[q]
                            for i in range(min(n, NDSEM)):
                                uses = (n - 1 - i) // NDSEM + 1
                                e.wait_ge(dsem[q][i], 16 * uses)
                return f

            with nc.Block() as block:
                block.sync(run("sync"))
                block.scalar(run("scalar"))
                block.vector(run("vector"))
                block.gpsimd(run("gpsimd"))
                block.tensor(run("tensor"))


D = 1024
SEQ = 16384
NCTX = 256
NT = SEQ + NCTX
DEPTH = 4
ALPHA = (2.0 * DEPTH) ** 0.25
LN_EPS = 1e-5
NEXP = 64
N_CORES = 1
MOE_EXPERTS_DECL = None
MOE_GROUP_LIMIT = None
DBG = set()
BLOCKS = [(0, NCTX, True)] + [(NCTX + i * 512, 512, False) for i in range(SEQ // 512)]


def _bcast_row(ap_row, nparts, n):
    return bass.AP(ap_row.tensor, int(ap_row.offset), [[0, nparts], [1, n]])


class Ctx:
    pass


def build_program(n_layers=DEPTH, stop_after=None):
    nc = bass.Bass("TRN2", target_bir_lowering=False)
    K = Ctx()
    K.nc = nc
    K.in_names = []

    def din(name, shape, dt=F32):
        K.in_names.append(name)
        return nc.dram_tensor(name, shape, dt, kind="ExternalInput").ap()

    K.din = din
    P = Prog(nc)
    K.P = P
    K.x = din("x", [SEQ, D]); K.ctx = din("ctx", [NCTX, D])
    K.cS = din("cS", [128, 8, 2])
    K.ident_in = din("ident", [128, 128]); K.oh4_in = din("oh4", [4, 4, 128])
    K.out = nc.dram_tensor("out", [SEQ, D], F32, kind="ExternalOutput").ap()
    K.HS = nc.dram_tensor("HS", [NT, D], F32, kind="Internal").ap()
    K.UT = nc.dram_tensor("UT", [D, NT], BF16, kind="Internal").ap()
    K.ZT = nc.dram_tensor("ZT", [2 * D, NT], BF16, kind="Internal").ap()
    K.YS = nc.dram_tensor("YS", [NT, D], F32, kind="Internal").ap()
    K.WT = nc.dram_tensor("WT", [NEXP, NT], F32, kind="Internal").ap()
    K.modrow = nc.dram_tensor("modrow", [DEPTH, 2, 6 * D], F32, kind="Internal").ap()
    K.ps = [nc.alloc_psum_tensor("ps%d" % i, [128, 512], F32) for i in range(8)]
    K.modT = nc.alloc_sbuf_tensor("modT", [128, DEPTH, 48, 2], F32)
    K.sc1p = nc.alloc_sbuf_tensor("sc1p", [128, DEPTH, 8, 2], F32)
    K.ident = nc.alloc_sbuf_tensor("identS", [128, 128], F32)
    K.oh4 = nc.alloc_sbuf_tensor("oh4S", [4, 4, 128], F32)
    P.dma(K.ident[:], K.ident_in)
    P.dma(K.oh4[:], K.oh4_in)

    layers = list(range(n_layers))
    K.scrA = nc.dram_tensor("scrA", [2 * D, NT], F32, kind="Internal").ap()
    K.scrB = nc.dram_tensor("scrB", [D, NT], F32, kind="Internal").ap()
    K.scrC = nc.dram_tensor("scrC", [D, NT], F32, kind="Internal").ap()
    K.ln_g = din("post_ln_g", [DEPTH, 2, D]); K.ln_b = din("post_ln_b", [DEPTH, 2, D])
    K.router = din("moe_router", [DEPTH, D, NEXP]); K.rbias = din("moe_bias", [DEPTH, NEXP])
    K.moe_w_gu = {}; K.moe_w_down = {}; K.moe_sh_gu = {}; K.moe_sh_down = {}; K.w_out = {}
    for l in layers:
        K.moe_w_gu[l] = din("moe_w_gu%d" % l, [MOE_EXPERTS_DECL or NEXP, D, 512])
        K.moe_w_down[l] = din("moe_w_down%d" % l, [MOE_EXPERTS_DECL or NEXP, 256, D])
        K.moe_sh_gu[l] = din("moe_sh_gu%d" % l, [D, 512])
        K.moe_sh_down[l] = din("moe_sh_down%d" % l, [256, D])
    if 0 in layers:
        K.lru_w_in = din("lru_w_in", [1, D, 2 * D]); K.lru_conv_w = din("lru_conv_w", [1, 128, 8, 4])
        K.lru_conv_b = din("lru_conv_b", [1, 128, 8]); K.lru_gate_w = din("lru_gate_w", [1, 2, 2, 8, 128, 128])
        K.lru_gate_b = din("lru_gate_b", [1, 128, 2, 2, 8]); K.lru_lam = din("lru_lam", [1, 128, 2, 8])
        K.w_out[0] = din("lru_w_out", [1, D, D])[0]
    for l in layers:
        if l % 4 != 0:
            declare_mixer_inputs(K, l)
    stage_mod(K, layers)
    stage_epilogue(K, layer=None, sub=None, nxt=(0, "mix"))
    done = False
    for l in layers:
        kind = l % 4
        dz = (mixer_lru, mixer_rwkv, mixer_ret, mixer_hgrn)[kind](K, l)
        last_mix = stop_after == (l, "mix")
        stage_epilogue(K, layer=l, sub="mix", nxt=None if (last_mix and "mixnxt" not in DBG) else (l, "ffn"), dz=dz, to_out=last_mix)
        if last_mix:
            done = True
            break
        if "nomoe" not in DBG:
            stage_moe(K, l)
        last = (l == DEPTH - 1) or stop_after == (l, "ffn")
        stage_epilogue(K, layer=l, sub="ffn", nxt=None if last else (l + 1, "mix"), to_out=last)
        if last:
            done = True
            break
    assert done
    P.emit()
    K.untracked = P.untracked
    return nc, K


def stage_mod(K, layers):
    nc, P, ps = K.nc, K.P, K.ps
    ada_w = K.din("ada_w", [DEPTH, D, 6 * D]); ada_b = K.din("ada_b", [128, DEPTH, 48])
    P.untracked.update(K.in_names)
    with ExitStack() as st:
        S = st.enter_context(nc.sbuf_tensor("S", [128, 8, 2], F32))
        adab = st.enter_context(nc.sbuf_tensor("adab", [128, DEPTH, 48], F32))
        aw = [st.enter_context(nc.sbuf_tensor("aw%d" % i, [128, 8, 512], F32)) for i in range(2)]
        mtr = st.enter_context(nc.sbuf_tensor("mtr", [48, 2, 128], F32))
        P.dma(S[:], K.cS)
        P.dma(adab[:], ada_b)
        P.act(S[:], S[:], AF.Silu)
        i = 0
        for l in layers:
            awv = ada_w[l].rearrange("(k p) n -> p k n", p=128)
            pb = ps[l % 2]
            for nb in range(12):
                buf = aw[i % 2]; i += 1
                P.dma(buf[:], awv[:, :, nb * 512:(nb + 1) * 512])
                for j in range(4):
                    cb = nb * 4 + j
                    for k in range(8):
                        P.mm(pb[:, cb * 2:cb * 2 + 2], buf[:, k, j * 128:(j + 1) * 128], S[:, k, :],
                             start=(k == 0), stop=(k == 7))
            psv = pb[:, 0:96].rearrange("p (c t) -> p c t", t=2)
            for t in range(2):
                P.tt(K.modT[:, l, :, t], psv[:, :, t], adab[:, l, :], ALU.add)
            P.ts(K.sc1p[:, l], K.modT[:, l, 8:16, :], 1.0, None, ALU.add)
            for t in range(2):
                P.transpose(ps[2][0:48, t * 128:(t + 1) * 128], K.modT[:, l, :, t], K.ident[:])
            P.copy(mtr[:].rearrange("c t p -> c (t p)"), ps[2][0:48, 0:256])
            for t in range(2):
                P.dma(K.modrow[l, t].rearrange("(c p) -> c p", p=128), mtr[:, t, :])
    P.barrier()


def stage_epilogue(K, layer, sub, nxt, dz=None, to_out=False, from_hs=False):
    if layer is not None and nxt is not None and "fusedep" not in DBG:
        stage_epilogue(K, layer, sub, None, dz=dz, to_out=to_out)
        stage_epilogue(K, None, None, nxt, from_hs=True)
        return
    nc, P, ps = K.nc, K.P, K.ps
    l = layer
    j0 = 0 if sub == "mix" else 3
    with ExitStack() as st:
        def sb(name, shape, dt=F32):
            K.uid = getattr(K, "uid", 0) + 1
            return st.enter_context(nc.sbuf_tensor("%s_%d" % (name, K.uid), shape, dt))
        if l is not None:
            gateB = sb("gateB", [128, 2, D]); gB = sb("gB", [128, D]); bB = sb("bB", [128, D])
            ln_g = K.ln_g[l]; ln_b = K.ln_b[l]
            si = 0 if sub == "mix" else 1
            for t in range(2):
                P.dma(gateB[:, t, :], _bcast_row(K.modrow[l, t:t + 1, (j0 + 2) * D:(j0 + 3) * D], 128, D))
            P.dma(gB[:], _bcast_row(ln_g[si:si + 1, :], 128, D))
            P.dma(bB[:], _bcast_row(ln_b[si:si + 1, :], 128, D))
            if sub == "mix":
                wo = sb("wo", [128, dz // 128, D], BF16)
                P.dma(wo[:], K.w_out[l].rearrange("(k p) n -> p k n", p=128), eng="gpsimd")
                zt = [sb("zt%d" % i, [128, dz // 128, 128], BF16) for i in range(2)]
                ZTv = K.ZT[0:dz, :].rearrange("(k p) t -> p k t", p=128)
            else:
                yt = [sb("yt%d" % i, [128, D]) for i in range(2)]
        if nxt is not None:
            nl, nsub = nxt
            nj = 0 if nsub == "mix" else 3
            shB = sb("shB", [128, 2, D]); scB = sb("scB", [128, 2, D])
            for t in range(2):
                P.dma(shB[:, t, :], _bcast_row(K.modrow[nl, t:t + 1, nj * D:(nj + 1) * D], 128, D))
                P.dma(scB[:, t, :], _bcast_row(K.modrow[nl, t:t + 1, (nj + 1) * D:(nj + 2) * D], 128, D))
                P.ts(scB[:, t, :], scB[:, t, :], 1.0, None, ALU.add)
            ub = sb("ub", [128, D])
            utb = [sb("utb%d" % i, [128, 8, 128], BF16) for i in range(2)]
            UTv = K.UT.rearrange("(k p) t -> p k t", p=128)
            if nsub == "ffn":
                utf = sb("utf", [128, 8, 128])
                rw = sb("rw", [128, 8, NEXP])
                rbias = sb("rbias", [128, NEXP])
                if "norw" not in DBG:
                    P.dma(rw[:], K.router[nl].rearrange("(k p) e -> p k e", p=128))
                    P.dma(rbias[:], _bcast_row(K.rbias[nl:nl + 1, :], 128, NEXP))
                sco = sb("sco", [128, NEXP]); cho = sb("cho", [128, NEXP]); mc = sb("mc", [128, NEXP])
                m8 = sb("m8", [128, 8, 8]); gs = sb("gs", [128, 8]); g8 = sb("g8", [128, 8]); gm = sb("gm", [128, 8])
                t8 = sb("t8", [128, 8]); wsel = sb("wsel", [128, NEXP]); ssum = sb("ssum", [128, 2])
                wtS = [sb("wtS%d" % i, [NEXP, 128]) for i in range(2)]
        ht = [sb("ht%d" % i, [128, D]) for i in range(2)]
        tt_ = sb("tt", [128, D]); junk = sb("junk", [128, D])
        ot = [sb("ot%d" % i, [128, D]) for i in range(2)]
        st1 = sb("st1", [128, 4])

        last_layer_noctx = (l == DEPTH - 1)
        for ti in range(NT // 128):
            isctx = ti < 2
            if isctx and (last_layer_noctx or (to_out and l is not None) or ("skipctx" in DBG and l is not None)):
                continue
            t = 1 if isctx else 0
            tok0 = ti * 128
            h = ht[ti % 2]; o = ot[ti % 2]
            if l is None:
                src = K.ctx[tok0:tok0 + 128, :] if isctx else K.x[tok0 - NCTX:tok0 - NCTX + 128, :]
                if from_hs:
                    src = K.HS[tok0:tok0 + 128, :]
                P.dma(o[:], src)
            else:
                P.dma(h[:], K.HS[tok0:tok0 + 128, :])
                if sub == "mix":
                    z = zt[ti % 2]
                    P.dma(z[:], ZTv[:, :, tok0:tok0 + 128])
                    nk = dz // 128
                    for nb in range(2):
                        for k in range(nk):
                            P.mm(ps[nb][:, :], z[:, k, :], wo[:, k, nb * 512:(nb + 1) * 512],
                                 start=(k == 0), stop=(k == nk - 1))
                    for nb in range(2):
                        P.tt(tt_[:, nb * 512:(nb + 1) * 512], ps[nb][:, :], gateB[:, t, nb * 512:(nb + 1) * 512], ALU.mult)
                else:
                    y = yt[ti % 2]
                    P.dma(y[:], K.YS[tok0:tok0 + 128, :])
                    P.tt(tt_[:], y[:], gateB[:, t, :], ALU.mult)
                P.stt(tt_[:], h[:], ALPHA, tt_[:], ALU.mult, ALU.add)
                P.reduce(st1[:, 0:1], tt_[:], ALU.add)
                P.ts(st1[:, 1:2], st1[:, 0:1], 1.0 / D, None, ALU.mult)
                P.ts(tt_[:], tt_[:], st1[:, 1:2], None, ALU.subtract)
                P.act(junk[:], tt_[:], AF.Square, accum_out=st1[:, 2:3])
                P.ts(st1[:, 3:4], st1[:, 2:3], 1.0 / D, LN_EPS, ALU.mult, ALU.add)
                P.act(st1[:, 3:4], st1[:, 3:4], AF.Sqrt)
                P._add("vector", lambda e, a=st1[:, 3:4]: e.reciprocal(a, a), [st1[:, 3:4]], [st1[:, 3:4]])
                P.stt(o[:], tt_[:], st1[:, 3:4], gB[:], ALU.mult, ALU.mult)
                P.tt(o[:], o[:], bB[:], ALU.add, eng="gpsimd")
            if to_out:
                P.dma(K.out[tok0 - NCTX:tok0 - NCTX + 128, :], o[:])
                if nxt is None:
                    continue
            if not from_hs:
                P.dma(K.HS[tok0:tok0 + 128, :], o[:])
            if nxt is None:
                continue
            if nl == DEPTH - 1 and nsub == "ffn" and isctx:
                continue
            P.tt(ub[:], o[:], scB[:, t, :], ALU.mult)
            P.tt(ub[:], ub[:], shB[:, t, :], ALU.add, eng="gpsimd")
            utile = utb[ti % 2]
            for hb in range(2):
                for k in range(4):
                    kk = hb * 4 + k
                    P.transpose(ps[2 + hb][:, k * 128:(k + 1) * 128], ub[:, kk * 128:(kk + 1) * 128], K.ident[:])
                if nsub == "ffn":
                    P.copy(utf[:, hb * 4:(hb + 1) * 4, :].rearrange("p k t -> p (k t)"), ps[2 + hb][:, :], eng="scalar")
                    P.copy(utile[:, hb * 4:(hb + 1) * 4, :].rearrange("p k t -> p (k t)"),
                           utf[:, hb * 4:(hb + 1) * 4, :].rearrange("p k t -> p (k t)"), eng="gpsimd")
                else:
                    P.copy(utile[:, hb * 4:(hb + 1) * 4, :].rearrange("p k t -> p (k t)"), ps[2 + hb][:, :], eng="scalar")
            P.dma(UTv[:, :, tok0:tok0 + 128], utile[:])
            if nsub != "ffn" or "norouter" in DBG:
                continue
            for k in range(8):
                P.mm(ps[4][:, 0:NEXP], utf[:, k, :], rw[:, k, :], start=(k == 0), stop=(k == 7))
            P.act(sco[:], ps[4][:, 0:NEXP], AF.Sigmoid)
            P.tt(cho[:], sco[:], rbias[:], ALU.add)
            for g in range(8):
                P._add("vector", lambda e, a=m8[:, g, :], b=cho[:, g * 8:(g + 1) * 8]: e.max(a, b),
                       [cho[:, g * 8:(g + 1) * 8]], [m8[:, g, :]])
            P.tt(gs[:], m8[:, :, 0], m8[:, :, 1], ALU.add)
            P._add("vector", lambda e, a=g8[:], b=gs[:]: e.max(a, b), [gs[:]], [g8[:]])
            P.ts(gm[:], gs[:], g8[:, 3:4], None, ALU.is_ge)
            P.ts(mc[:], cho[:], 2.0, None, ALU.add)
            P.tt(mc[:].rearrange("p (g e) -> p g e", e=8), mc[:].rearrange("p (g e) -> p g e", e=8),
                 gm[:].unsqueeze(2).to_broadcast([128, 8, 8]), ALU.mult)
            P.ts(mc[:], mc[:], -2.0, None, ALU.add)
            P._add("vector", lambda e, a=t8[:], b=mc[:]: e.max(a, b), [mc[:]], [t8[:]])
            P.ts(wsel[:], mc[:], t8[:, 7:8], None, ALU.is_ge)
            P.tt(wsel[:], wsel[:], sco[:], ALU.mult)
            P.reduce(ssum[:, 0:1], wsel[:], ALU.add)
            P._add("vector", lambda e, a=ssum[:, 1:2], b=ssum[:, 0:1]: e.reciprocal(a, b), [ssum[:, 0:1]], [ssum[:, 1:2]])
            P.ts(wsel[:], wsel[:], ssum[:, 1:2], 2.5, ALU.mult, ALU.mult)
            P.transpose(ps[5][0:NEXP, 0:128], wsel[:], K.ident[:])
            w_ = wtS[ti % 2]
            P.copy(w_[:], ps[5][0:NEXP, 0:128], eng="scalar")
            P.dma(K.WT[:, tok0:tok0 + 128], w_[:])
    P.barrier()


def stage_moe(K, l):
    nc, P, ps = K.nc, K.P, K.ps
    w_gu = K.moe_w_gu[l]; w_dn = K.moe_w_down[l]; sh_gu = K.moe_sh_gu[l]; sh_dn = K.moe_sh_down[l]
    noctx = (l == DEPTH - 1)
    groups = [list(range(g * 4, g * 4 + 4)) for g in range(NEXP // 4)] + [[NEXP]]
    if MOE_GROUP_LIMIT is not None:
        groups = groups[:MOE_GROUP_LIMIT] + [[NEXP]]
    with ExitStack() as st:
        def sb(name, shape, dt=F32):
            K.uid = getattr(K, "uid", 0) + 1
            return st.enter_context(nc.sbuf_tensor("%s_%d" % (name, K.uid), shape, dt))
        wgu = [sb("wgu%d" % i, [128, 4, 8, 512], BF16) for i in range(2)]
        wdn = [sb("wdn%d" % i, [128, 4, 2, D], BF16) for i in range(2)]
        uT = [sb("uTm%d" % i, [128, 8, 512], BF16) for i in range(2)]
        wt4 = [sb("wt4%d" % i, [4, 512]) for i in range(2)]
        actT = [sb("actT%d" % i, [128, 4, 2, 512], BF16) for i in range(2)]
        tmp = [sb("tmpm%d" % i, [128, 512]) for i in range(2)]
        yb = [sb("yb%d" % i, [128, D]) for i in range(2)]
        UTv = K.UT.rearrange("(k p) t -> p k t", p=128)
        it = 0
        yi = 0
        for gi, grp in enumerate(groups):
            wg = wgu[gi % 2]; wd = wdn[gi % 2]
            for j, e in enumerate(grp):
                if e < NEXP:
                    P.dma(wg[:, j], w_gu[e].rearrange("(k p) n -> p k n", p=128), eng="gpsimd")
                    P.dma(wd[:, j], w_dn[e].rearrange("(k p) n -> p k n", p=128), eng="gpsimd")
                else:
                    P.dma(wg[:, j], sh_gu.rearrange("(k p) n -> p k n", p=128), eng="gpsimd")
                    P.dma(wd[:, j], sh_dn.rearrange("(k p) n -> p k n", p=128), eng="gpsimd")
            shared = grp[0] == NEXP
            for (s0, n, isctx) in BLOCKS:
                if isctx and noctx:
                    continue
                u = uT[it % 2]; a = actT[it % 2]; w4 = wt4[it % 2]
                it += 1
                P.dma(u[:, :, :n], UTv[:, :, s0:s0 + n])
                if not shared:
                    P.dma(w4[:, :n], K.WT[grp[0]:grp[0] + 4, s0:s0 + n])
                for j, e in enumerate(grp):
                    for ct in range(4):
                        for k in range(8):
                            P.mm(ps[ct][:, :n], wg[:, j, k, ct * 128:(ct + 1) * 128], u[:, k, :n],
                                 start=(k == 0), stop=(k == 7))
                    if not shared:
                        wb = ps[4 + j % 2]
                        P.mm(wb[:, :n], K.oh4[:, j, :], w4[:, :n])
                    for hc in range(2):
                        tm = tmp[hc]
                        P.act(tm[:, :n], ps[hc][:, :n], AF.Silu)
                        if shared:
                            P.tt(a[:, j, hc, :n], tm[:, :n], ps[2 + hc][:, :n], ALU.mult)
                        else:
                            P.tt(tm[:, :n], tm[:, :n], ps[2 + hc][:, :n], ALU.mult)
                            P.tt(a[:, j, hc, :n], tm[:, :n], wb[:, :n], ALU.mult)
                for tl in range(n // 128):
                    tok0 = s0 + tl * 128
                    y = yb[yi % 2]
                    yi += 1
                    if gi > 0:
                        P.dma(y[:], K.YS[tok0:tok0 + 128, :])
                    for half in range(2):
                        pb = ps[6 + half]
                        cnt = 0
                        tot = len(grp) * 2
                        for j in range(len(grp)):
                            for kc in range(2):
                                P.mm(pb[:, :], a[:, j, kc, tl * 128:(tl + 1) * 128], wd[:, j, kc, half * 512:(half + 1) * 512],
                                     start=(cnt == 0), stop=(cnt == tot - 1))
                                cnt += 1
                        if gi > 0:
                            P.tt(y[:, half * 512:(half + 1) * 512], pb[:, :], y[:, half * 512:(half + 1) * 512], ALU.add)
                        else:
                            P.copy(y[:, half * 512:(half + 1) * 512], pb[:, :], eng="scalar")
                    P.dma(K.YS[tok0:tok0 + 128, :], y[:])
    P.barrier()


def mixer_lru(K, l):
    nc, P, ps = K.nc, K.P, K.ps
    j = l // 4
    w_in = K.lru_w_in[j]; conv_w = K.lru_conv_w[j]; conv_b = K.lru_conv_b[j]
    gate_w = K.lru_gate_w[j]; gate_b = K.lru_gate_b[j]; lam = K.lru_lam[j]
    GT = K.scrA[0:D, :]; XT = K.scrB; HF = K.scrC
    ZT = K.ZT
    with ExitStack() as st:
        def sb(name, shape, dt=F32):
            K.uid = getattr(K, "uid", 0) + 1
            return st.enter_context(nc.sbuf_tensor("%s_%d" % (name, K.uid), shape, dt))
        win = sb("win", [128, 8, 2 * D], BF16)
        uT = [sb("uT%d" % i, [128, 8, 512], BF16) for i in range(2)]
        Gb = [sb("Gb%d" % i, [128, 8, 512]) for i in range(2)]
        Xb = [sb("Xb%d" % i, [128, 8, 512]) for i in range(2)]
        t1 = sb("t1", [128, 512]); t2 = sb("t2", [128, 512])
        P.dma(win[:], w_in.rearrange("(k p) n -> p k n", p=128), eng="gpsimd")
        UTv = K.UT.rearrange("(k p) t -> p k t", p=128)
        GTv = GT.rearrange("(k p) t -> p k t", p=128)
        XTv = XT.rearrange("(k p) t -> p k t", p=128)
        for bi, (s0, n, isctx) in enumerate(BLOCKS):
            u = uT[bi % 2]; G = Gb[bi % 2]; X = Xb[bi % 2]
            P.dma(u[:, :, :n], UTv[:, :, s0:s0 + n])
            for ct in range(16):
                pp = ps[2 + ct % 4]
                for k in range(8):
                    P.mm(pp[:, :n], win[:, k, ct * 128:(ct + 1) * 128], u[:, k, :n], start=(k == 0), stop=(k == 7))
                if ct < 8:
                    P.act(t1[:, :n], pp[:, :n], AF.Square)
                    P.ts(t1[:, :n], t1[:, :n], 0.044715, 1.0, ALU.mult, ALU.add)
                    P.tt(t1[:, :n], t1[:, :n], pp[:, :n], ALU.mult)
                    P.act(t2[:, :n], t1[:, :n], AF.Sigmoid, scale=1.5957691216)
                    P.tt(G[:, ct, :n], t2[:, :n], pp[:, :n], ALU.mult)
                else:
                    P.copy(X[:, ct - 8, :n], pp[:, :n], eng="scalar")
            P.dma(GTv[:, :, s0:s0 + n], G[:, :, :n])
            P.dma(XTv[:, :, s0:s0 + n], X[:, :, :n])
    P.barrier()
    with ExitStack() as st:
        def sb(name, shape, dt=F32):
            K.uid = getattr(K, "uid", 0) + 1
            return st.enter_context(nc.sbuf_tensor("%s_%d" % (name, K.uid), shape, dt))
        XP = sb("XP", [128, NT + 8])
        CO = 1; LO = 260
        cw = sb("cw", [128, 8, 4]); cb_ = sb("cb", [128, 8]); gb = sb("gb", [128, 2, 2, 8])
        lm = sb("lm", [128, 2, 8]); nsp8 = sb("nsp8", [128, 2, 8])
        gw = sb("gw", [128, 2, 2, 128], BF16)
        xc = sb("xc", [128, 512]); xcb = sb("xcb", [128, 512], BF16)
        rr = sb("rr", [128, 512]); ii = sb("ii", [128, 512]); aa = sb("aa", [128, 512]); bb = sb("bb", [128, 512])
        hh = sb("hh", [128, 512]); hf = sb("hf", [128, 512]); gg = sb("gg", [128, 512])
        zb = sb("zb", [128, 512], BF16); carry = sb("carry", [128, 1])
        P.dma(cw[:], conv_w); P.dma(cb_[:], conv_b); P.dma(gb[:], gate_b); P.dma(lm[:], lam)
        P.act(nsp8[:], lm[:], AF.Exp, scale=-1.0)
        P.act(nsp8[:], nsp8[:], AF.Ln, bias=1.0)
        P.ts(nsp8[:], nsp8[:], -8.0, None, ALU.mult)
        P.memset(XP[:], 0.0)
        for c in range(8):
            P.dma(XP[:, CO:CO + NCTX], XT[c * 128:(c + 1) * 128, 0:NCTX])
            P.dma(XP[:, LO:LO + SEQ], XT[c * 128:(c + 1) * 128, NCTX:NT])
            for d in range(2):
                for g in range(2):
                    P.dma(gw[:, d, g, :], gate_w[d, g, c], eng="gpsimd")
            for d in range(2):
                order = BLOCKS if d == 0 else ([BLOCKS[0]] + BLOCKS[:0:-1])
                P.memset(carry[:], 0.0)
                for (s0, n, isctx) in order:
                    base = (CO + s0) if isctx else (LO + s0 - NCTX)
                    P.ts(xc[:, :n], XP[:, base - 1:base - 1 + n], cw[:, c, 0:1], cb_[:, c:c + 1], ALU.mult, ALU.add)
                    for jj in range(1, 4):
                        P.stt(xc[:, :n], XP[:, base - 1 + jj:base - 1 + jj + n], cw[:, c, jj:jj + 1], xc[:, :n],
                              ALU.mult, ALU.add)
                    P.copy(xcb[:, :n], xc[:, :n], eng="gpsimd")
                    P.mm(ps[6][:, :n], gw[:, d, 0, :], xcb[:, :n])
                    P.mm(ps[7][:, :n], gw[:, d, 1, :], xcb[:, :n])
                    P.act(rr[:, :n], ps[6][:, :n], AF.Sigmoid, bias=gb[:, d, 0, c:c + 1])
                    P.act(ii[:, :n], ps[7][:, :n], AF.Sigmoid, bias=gb[:, d, 1, c:c + 1])
                    P.act(aa[:, :n], rr[:, :n], AF.Exp, scale=nsp8[:, d, c:c + 1])
                    P.tt(bb[:, :n], aa[:, :n], aa[:, :n], ALU.mult)
                    P.ts(bb[:, :n], bb[:, :n], -1.0, 1.0, ALU.mult, ALU.add)
                    P.ts(bb[:, :n], bb[:, :n], 0.0, None, ALU.max)
                    P.act(bb[:, :n], bb[:, :n], AF.Sqrt)
                    P.tt(ii[:, :n], ii[:, :n], xc[:, :n], ALU.mult, eng="gpsimd")
                    P.tt(bb[:, :n], bb[:, :n], ii[:, :n], ALU.mult)
                    if d == 0:
                        P.scan(hh[:, :n], aa[:, :n], bb[:, :n], carry[:, 0:1], ALU.mult, ALU.add)
                        P.copy(carry[:, 0:1], hh[:, n - 1:n], eng="scalar")
                        P.dma(HF[c * 128:(c + 1) * 128, s0:s0 + n], hh[:, :n])
                    else:
                        P.scan(hh[:, n - 1::-1], aa[:, n - 1::-1], bb[:, n - 1::-1], carry[:, 0:1], ALU.mult, ALU.add)
                        P.copy(carry[:, 0:1], hh[:, 0:1], eng="scalar")
                        P.dma(hf[:, :n], HF[c * 128:(c + 1) * 128, s0:s0 + n])
                        P.dma(gg[:, :n], GT[c * 128:(c + 1) * 128, s0:s0 + n])
                        P.tt(hh[:, :n], hh[:, :n], hf[:, :n], ALU.add)
                        P.tt(zb[:, :n], hh[:, :n], gg[:, :n], ALU.mult)
                        P.dma(ZT[c * 128:(c + 1) * 128, s0:s0 + n], zb[:, :n])
    P.barrier()
    return D


def declare_mixer_inputs(K, l):
    din = K.din
    kind = l % 4
    if kind == 3:
        K.hgrn_w_in = din("hgrn_w_in", [1, D, 5 * D]); K.hgrn_b_f = din("hgrn_b_f", [1, 128, 2, 8])
        K.hgrn_lb = din("hgrn_lb", [128, 8, 4]); K.hgrn_norm_g = din("hgrn_norm_g", [1, 128, 1])
        K.hg_masks = din("hg_masks", [128, 2, 128])
        if not hasattr(K, "w_out"):
            K.w_out = {}
        K.w_out[l] = din("hgrn_w_out", [1, D, D])[0]
    if kind == 1:
        K.rwkv_w_in = din("rwkv_w_in", [1, 3, D, D])
        K.rwkv_w_l1 = din("rwkv_w_l1", [1, 2, D, 64]); K.rwkv_w_l2 = din("rwkv_w_l2", [1, 2, 64, D])
        K.rwkv_a_l1 = din("rwkv_a_l1", [1, 2, D, 64]); K.rwkv_a_l2 = din("rwkv_a_l2", [1, 2, 64, D])
        K.rwkv_g_l1 = din("rwkv_g_l1", [1, D, 128]); K.rwkv_g_l2 = din("rwkv_g_l2", [1, 128, D])
        K.rwkv_vec = din("rwkv_vec", [1, 128, 16, 8]); K.rw_consts = din("rw_consts", [128, 5, 128])
        if not hasattr(K, "w_out"):
            K.w_out = {}
        K.w_out[l] = din("rwkv_w_out", [1, D, D])[0]
    if kind == 2:
        K.ret_w_in = din("ret_w_in", [1, D, 6 * D]); K.ret_decay = din("ret_decay", [1, 8])
        K.ret_gn_g = din("ret_gn_g", [1, 128, 16]); K.ret_gn_b = din("ret_gn_b", [1, 128, 16])
        K.ret_consts = din("ret_consts", [128, 5, 128]); K.ret_poscol = din("ret_poscol", [128, 2])
        K.rope_cos = din("rope_cos", [128, SEQ]); K.rope_sin = din("rope_sin", [128, SEQ])
        if not hasattr(K, "w_out"):
            K.w_out = {}
        K.w_out[l] = din("ret_w_out", [1, 2 * D, D])[0]

_CACHE = {}


def _pk(v):
    return np.ascontiguousarray(np.asarray(v, np.float32).reshape(-1, 128).T)


def host_inputs(inp, names):
    f32 = np.float32
    A = lambda v: np.ascontiguousarray(np.asarray(v, f32))
    oh4 = np.zeros((4, 4, 128), f32)
    for j in range(4):
        oh4[j, j, :] = 1.0
    d = {"ident": np.eye(128, dtype=f32), "oh4": oh4}
    if "x" in names:
        d["x"] = A(inp["x"][0]); d["ctx"] = A(inp["ctx"][0])
        d["cS"] = A(np.stack([_pk(inp["c"][0]), _pk(inp["c_ctx"])], axis=-1))
        d["ada_w"] = A(inp["ada_w"])
        d["ada_b"] = A(np.asarray(inp["ada_b"], f32).reshape(DEPTH, 48, 128).transpose(2, 0, 1))
        d["post_ln_g"] = A(inp["post_ln_g"]); d["post_ln_b"] = A(inp["post_ln_b"])
        d["moe_router"] = A(inp["moe_router"]); d["moe_bias"] = A(inp["moe_bias"])
    for l in range(DEPTH):
        if "moe_w_gu%d" % l in names:
            ne = MOE_EXPERTS_DECL or NEXP
            d["moe_w_gu%d" % l] = A(inp["moe_w_gu"][l][:ne]); d["moe_w_down%d" % l] = A(inp["moe_w_down"][l][:ne])
            d["moe_sh_gu%d" % l] = A(inp["moe_sh_gu"][l]); d["moe_sh_down%d" % l] = A(inp["moe_sh_down"][l])
    if "lru_w_in" in names:
        g_in = np.asarray(inp["lru_gate_w"], f32)
        gw = np.zeros((1, 2, 2, 8, 128, 128), f32)
        for dd in range(2):
            for g in range(2):
                for t in range(8):
                    gw[0, dd, g, t, 0:64, 0:64] = g_in[0, dd, g, 2 * t]
                    gw[0, dd, g, t, 64:128, 64:128] = g_in[0, dd, g, 2 * t + 1]
        d["lru_w_in"] = A(inp["lru_w_in"]); d["lru_w_out"] = A(inp["lru_w_out"])
        d["lru_conv_w"] = A(np.asarray(inp["lru_conv_w"], f32)[0].T.reshape(8, 128, 4).transpose(1, 0, 2)[None])
        d["lru_conv_b"] = A(_pk(inp["lru_conv_b"][0])[None])
        d["lru_gate_w"] = gw
        d["lru_gate_b"] = A(np.asarray(inp["lru_gate_b"], f32)[0].reshape(2, 2, 8, 128).transpose(3, 0, 1, 2)[None])
        d["lru_lam"] = A(np.asarray(inp["lru_lambda"], f32)[0].reshape(2, 8, 128).transpose(2, 0, 1)[None])
    if "hgrn_w_in" in names:
        d["hgrn_w_in"] = A(inp["hgrn_w_in"]); d["hgrn_w_out"] = A(inp["hgrn_w_out"])
        d["hgrn_b_f"] = A(np.asarray(inp["hgrn_b_f"], f32).reshape(1, 2, 8, 128).transpose(0, 3, 1, 2))
        d["hgrn_lb"] = A(np.asarray(inp["hgrn_lb"], f32).reshape(DEPTH, 8, 128).transpose(2, 1, 0))
        d["hgrn_norm_g"] = A(np.asarray(inp["hgrn_norm_g"], f32).reshape(1, 128, 1))
        m = np.zeros((128, 2, 128), f32)
        for s_ in range(128):
            for t_ in range(128):
                if s_ // 64 == t_ // 64:
                    m[s_, 0, t_] = 1.0 if s_ <= t_ else 0.0
                    m[s_, 1, t_] = 1.0 if s_ >= t_ else 0.0
        d["hg_masks"] = m
    if "ret_w_in" in names:
        d["ret_w_in"] = A(inp["ret_w_in"]); d["ret_w_out"] = A(inp["ret_w_out"])
        d["ret_decay"] = A(np.asarray(inp["ret_decay"], f32).reshape(1, 8))
        d["ret_gn_g"] = A(np.asarray(inp["ret_gn_g"], f32).reshape(1, 16, 128).transpose(0, 2, 1))
        d["ret_gn_b"] = A(np.asarray(inp["ret_gn_b"], f32).reshape(1, 16, 128).transpose(0, 2, 1))
        ss, tt = np.meshgrid(np.arange(128), np.arange(128), indexing="ij")
        cst = np.zeros((128, 5, 128), f32)
        cst[:, 0, :] = (ss <= tt); cst[:, 1, :] = (ss >= tt); cst[:, 2, :] = np.abs(tt - ss)
        cst[:, 3, :] = tt + 1; cst[:, 4, :] = 128 - tt
        d["ret_consts"] = cst
        d["ret_poscol"] = A(np.stack([127 - np.arange(128), np.arange(128)], axis=1))
        rows_ = SEQ // 64
        pos_row = np.repeat(np.arange(rows_, dtype=f32), 64); pos_col = np.tile(np.arange(64, dtype=f32), rows_)
        freqs = (f32(10000.0) ** (-np.arange(64, dtype=f32) / f32(64))).astype(f32)
        ang = np.concatenate([pos_row[:, None] * freqs, pos_col[:, None] * freqs], axis=-1).astype(f32)
        d["rope_cos"] = A(np.cos(ang).T); d["rope_sin"] = A(np.sin(ang).T)
    if "rwkv_w_in" in names:
        for k_ in ("rwkv_w_in", "rwkv_w_l1", "rwkv_w_l2", "rwkv_a_l1", "rwkv_a_l2", "rwkv_g_l1", "rwkv_g_l2", "rwkv_w_out"):
            d[k_] = A(inp[k_])
        ka_ = np.asarray(inp["rwkv_k_a"], f32)[0]
        rows_ = [np.asarray(inp["rwkv_mu"], f32)[0][i] for i in range(6)]
        rows_ += [np.asarray(inp["rwkv_w0"], f32)[0][z_] for z_ in range(2)]
        rows_ += [np.asarray(inp["rwkv_a0"], f32)[0][z_] for z_ in range(2)]
        rows_ += [np.asarray(inp["rwkv_k_k"], f32)[0], ka_, np.asarray(inp["rwkv_r_k"], f32)[0].reshape(-1),
                  np.asarray(inp["rwkv_ln_g"], f32)[0], np.asarray(inp["rwkv_ln_b"], f32)[0], ka_]
        vec = np.stack([_pk(r_) for r_ in rows_], axis=1)
        d["rwkv_vec"] = A(vec[None])
        tt_, ii_ = np.meshgrid(np.arange(128), np.arange(128), indexing="ij")
        same = (tt_ // 64 == ii_ // 64)
        cst = np.zeros((128, 5, 128), f32)
        cst[:, 0, :] = same & (ii_ < tt_); cst[:, 1, :] = same & (ii_ > tt_)
        cst[:, 2, :] = same & (tt_ <= ii_); cst[:, 3, :] = same & (tt_ >= ii_); cst[:, 4, :] = same
        d["rw_consts"] = cst
    return {k: d[k] for k in names if k in d}


def kernel(_n_layers=DEPTH, _stop_after=None, **inp):
    key = (_n_layers, _stop_after)
    if key not in _CACHE:
        _CACHE[key] = build_program(_n_layers, _stop_after)
    nc, K = _CACHE[key]
    real = host_inputs(inp, K.in_names)
    ncores = N_CORES
    maps = [real] + [{k: np.zeros_like(v) for k, v in real.items()} for _ in range(ncores - 1)]
    res = run_bass_kernel_spmd(nc, maps, core_ids=list(range(ncores)))
    return np.asarray(res.results[0]["out"], dtype=np.float32)[None]


def mixer_hgrn(K, l):
    nc, P, ps = K.nc, K.P, K.ps
    j = l // 4
    w_in = K.hgrn_w_in[j]; b_f = K.hgrn_b_f[j]; norm_g = K.hgrn_norm_g[j]
    OF = K.scrA
    C = 64
    with ExitStack() as st:
        def sb(name, shape, dt=F32):
            K.uid = getattr(K, "uid", 0) + 1
            return st.enter_context(nc.sbuf_tensor("%s_%d" % (name, K.uid), shape, dt))
        win = sb("hwin", [128, 8, 5 * D], BF16)
        P.dma(win[:], w_in.rearrange("(k p) n -> p k n", p=128), eng="gpsimd")
        bf = sb("hbf", [128, 2, 8]); lbr = sb("hlbr", [128, 8, 4]); lb = sb("hlb", [128, 8]); oml = sb("homl", [128, 8])
        ssum = sb("hss", [128, 8]); ng = sb("hng", [128, 1])
        P.dma(bf[:], b_f); P.dma(lbr[:], K.hgrn_lb); P.dma(ng[:], norm_g)
        P.act(lbr[:], lbr[:], AF.Exp)
        P.reduce(ssum[:], lbr[:], ALU.add)
        P.reduce(lb[:], lbr[:, :, 1:l + 1], ALU.add)
        P._add("vector", lambda e, a=ssum[:]: e.reciprocal(a, a), [ssum[:]], [ssum[:]])
        P.tt(lb[:], lb[:], ssum[:], ALU.mult)
        P.ts(oml[:], lb[:], -1.0, 1.0, ALU.mult, ALU.add)
        masks = sb("hmask", [128, 2, 128]); P.dma(masks[:], K.hg_masks)
        nst = sb("hnst", [128, 2, 512])
        P.memset(nst[:], 1.0)
        P.memset(nst[:, 0, 0:512:C], 0.0)
        P.memset(nst[:, 1, C - 1:512:C], 0.0)
        ones = sb("hones", [128, 128]); P.memset(ones[:], 1.0)
        S = sb("hS", [128, 8, 128]); Sb = sb("hSb", [128, 8, 128], BF16)
        u = [sb("hu%d" % i, [128, 8, 512], BF16) for i in range(2)]
        qh = sb("hq", [128, 512]); fh = sb("hf", [128, 512]); lf = sb("hlf", [128, 512]); kh = sb("hk", [128, 512])
        vT = sb("hvT", [128, 512]); B = sb("hB", [128, 512]); e1 = sb("he1", [128, 512])
        qt = sb("hqt", [128, 512], BF16); kt = sb("hkt", [128, 512], BF16); khT = sb("hkhT", [128, 512])
        V = sb("hV", [128, 4, 128], BF16); Kh = sb("hKh", [128, 4, 128], BF16)
        eBl = sb("heBl", [128, 8]); AmT = sb("hAmT", [128, 128], BF16); tmpI = sb("htI", [128, 128])
        oT = sb("hoT", [128, 512]); of = sb("hof", [128, 512]); gT = sb("hgT", [128, 512])
        rs = sb("hrs", [128, 512]); zb = sb("hzb", [128, 512], BF16)
        UTv = K.UT.rearrange("(k p) t -> p k t", p=128)

        def proj(dst_ps, col0, uu, n):
            for k in range(8):
                P.mm(dst_ps[:, :n], win[:, k, col0:col0 + 128], uu[:, k, :n], start=(k == 0), stop=(k == 7))

        it = 0
        for d in range(2):
            order = BLOCKS if d == 0 else ([BLOCKS[0]] + BLOCKS[:0:-1])
            P.memset(S[:], 0.0); P.memset(Sb[:], 0.0)
            for (s0, n, isctx) in order:
                uu = u[it % 2]; it += 1
                P.dma(uu[:, :, :n], UTv[:, :, s0:s0 + n])
                nch = n // C
                for h in range(8):
                    proj(ps[0], h * 128, uu, n)
                    P.act(qh[:, :n], ps[0][:, :n], AF.Silu)
                    proj(ps[1], (1 + d) * D + h * 128, uu, n)
                    P.act(fh[:, :n], ps[1][:, :n], AF.Sigmoid, bias=bf[:, d, h:h + 1])
                    P.ts(fh[:, :n], fh[:, :n], oml[:, h:h + 1], lb[:, h:h + 1], ALU.mult, ALU.add)
                    P.act(lf[:, :n], fh[:, :n], AF.Ln)
                    P.ts(kh[:, :n], fh[:, :n], -1.0, 1.0, ALU.mult, ALU.add)
                    proj(ps[0], 3 * D + h * 128, uu, n)
                    P.copy(vT[:, :n], ps[0][:, :n], eng="scalar")
                    for pi in range(n // 128):
                        P.transpose(ps[2][:, pi * 128:(pi + 1) * 128], vT[:, pi * 128:(pi + 1) * 128], K.ident[:])
                    P.copy(V[:, :n // 128, :].rearrange("p a b -> p (a b)"), ps[2][:, :n], eng="scalar")
                    if d == 0:
                        P.scan(B[:, :n], nst[:, 0, :n], lf[:, :n], 0.0, ALU.mult, ALU.add)
                    else:
                        P.scan(B[:, n - 1::-1], nst[:, 1, n - 1::-1], lf[:, n - 1::-1], 0.0, ALU.mult, ALU.add)
                    P.act(e1[:, :n], B[:, :n], AF.Exp)
                    P.tt(qt[:, :n], qh[:, :n], e1[:, :n], ALU.mult)
                    P.act(e1[:, :n], B[:, :n], AF.Exp, scale=-1.0)
                    P.tt(kt[:, :n], kh[:, :n], e1[:, :n], ALU.mult)
                    Bv = B[:, :n].rearrange("p (c t) -> p c t", t=C)
                    last = C - 1 if d == 0 else 0
                    Bl = Bv[:, :, last:last + 1]
                    P.act(eBl[:, :nch], Bv[:, :, last], AF.Exp)
                    P.tt(e1[:, :n].rearrange("p (c t) -> p c t", t=C), Bv, Bl.to_broadcast([128, nch, C]), ALU.subtract)
                    P.act(e1[:, :n], e1[:, :n], AF.Exp, scale=-1.0)
                    P.tt(khT[:, :n], kh[:, :n], e1[:, :n], ALU.mult)
                    for pi in range(n // 128):
                        P.transpose(ps[2][:, pi * 128:(pi + 1) * 128], khT[:, pi * 128:(pi + 1) * 128], K.ident[:])
                    P.copy(Kh[:, :n // 128, :].rearrange("p a b -> p (a b)"), ps[2][:, :n], eng="scalar")
                    prs = list(range(n // 128)) if d == 0 else list(range(n // 128 - 1, -1, -1))
                    for pi in prs:
                        c128 = slice(pi * 128, (pi + 1) * 128)
                        P.mm(ps[3][:, 0:128], kt[:, c128], qt[:, c128])
                        P.tt(AmT[:], ps[3][:, 0:128], masks[:, d, :], ALU.mult)
                        P.mm(ps[4][:, 0:128], V[:, pi, :], AmT[:])
                        for c in ([0, 1] if d == 0 else [1, 0]):
                            cs = slice(pi * 128 + c * C, pi * 128 + (c + 1) * C)
                            rows = slice(c * C, (c + 1) * C)
                            P.mm(ps[5][:, c * C:(c + 1) * C], Sb[:, h, :], qt[:, cs])
                            P.mm(ps[6][:, 0:128], Kh[rows, pi, :], V[rows, pi, :])
                            ch = pi * 2 + c
                            P.stt(S[:, h, :], S[:, h, :], eBl[:, ch:ch + 1], ps[6][:, 0:128], ALU.mult, ALU.add)
                            P.copy(Sb[:, h, :], S[:, h, :], eng="gpsimd")
                        P.copy(tmpI[:], ps[5][:, 0:128], eng="scalar")
                        P.tt(oT[:, c128], ps[4][:, 0:128], tmpI[:], ALU.add)
                    if d == 0:
                        P.dma(OF[h * 128:(h + 1) * 128, s0:s0 + n], oT[:, :n])
                    else:
                        P.dma(of[:, :n], OF[h * 128:(h + 1) * 128, s0:s0 + n])
                        P.tt(oT[:, :n], oT[:, :n], of[:, :n], ALU.add)
                        P.act(rs[:, :n], oT[:, :n], AF.Square)
                        P.mm(ps[7][:, :n], ones[:], rs[:, :n])
                        P.ts(rs[:, :n], ps[7][:, :n], 1.0 / 128, LN_EPS, ALU.mult, ALU.add)
                        P.act(rs[:, :n], rs[:, :n], AF.Sqrt)
                        P._add("vector", lambda e, a=rs[:, :n]: e.reciprocal(a, a), [rs[:, :n]], [rs[:, :n]])
                        P.stt(oT[:, :n], oT[:, :n], ng[:, 0:1], rs[:, :n], ALU.mult, ALU.mult)
                        proj(ps[1], 4 * D + h * 128, uu, n)
                        P.act(gT[:, :n], ps[1][:, :n], AF.Silu)
                        P.tt(zb[:, :n], oT[:, :n], gT[:, :n], ALU.mult)
                        P.dma(K.ZT[h * 128:(h + 1) * 128, s0:s0 + n], zb[:, :n])
    P.barrier()
    return D


def mixer_ret(K, l):
    nc, P, ps = K.nc, K.P, K.ps
    j = l // 4
    w_in = K.ret_w_in[j]
    OF = K.scrA
    C = 128
    with ExitStack() as st:
        def sb(name, shape, dt=F32):
            K.uid = getattr(K, "uid", 0) + 1
            return st.enter_context(nc.sbuf_tensor("%s_%d" % (name, K.uid), shape, dt))
        win = sb("rwin", [128, 8, 6 * D], BF16)
        P.dma(win[:], w_in.rearrange("(k p) n -> p k n", p=128), eng="gpsimd")
        cst = sb("rcst", [128, 5, 128]); P.dma(cst[:], K.ret_consts)
        pcol = sb("rpcol", [128, 2]); P.dma(pcol[:], K.ret_poscol)
        lg = sb("rlg", [128, 8]); P.dma(lg[:], _bcast_row(K.ret_decay[j:j + 1, :], 128, 8))
        gng = sb("rgng", [128, 16]); gnb = sb("rgnb", [128, 16])
        P.dma(gng[:], K.ret_gn_g[j]); P.dma(gnb[:], K.ret_gn_b[j])
        P.act(lg[:], lg[:], AF.Sigmoid)
        P.act(lg[:], lg[:], AF.Ln)
        Dm = sb("rDm", [128, 8, 128]); QD = sb("rQD", [128, 8, 128]); kdc = sb("rkdc", [128, 8]); g128 = sb("rg128", [128, 8])
        for dh in range(8):
            d = dh // 4
            P.act(Dm[:, dh, :], cst[:, 2, :], AF.Exp, scale=lg[:, dh:dh + 1])
            P.tt(Dm[:, dh, :], Dm[:, dh, :], cst[:, d, :], ALU.mult)
            P.act(QD[:, dh, :], cst[:, 3 + d, :], AF.Exp, scale=lg[:, dh:dh + 1])
            P.act(kdc[:, dh:dh + 1], pcol[:, d:d + 1], AF.Exp, scale=lg[:, dh:dh + 1])
        P.act(g128[:], lg[:], AF.Exp, scale=float(C))
        ones = sb("rones", [128, 128]); P.memset(ones[:], 1.0)
        S = sb("rS", [128, 4, 2, 512]); Sb = sb("rSb", [128, 4, 2, 512], BF16)
        u = [sb("ru0", [128, 8, 512], BF16)] * 2
        cs = sb("rcos", [128, 512]); sn = sb("rsin", [128, 512])
        qT = sb("rqT", [128, 2, 512]); kT = sb("rkT", [128, 2, 512]); r1 = sb("rr1", [128, 512]); r2 = sb("rr2", [128, 512])
        qR = sb("rqR", [128, 2, 512]); kR = sb("rkR", [128, 2, 512])
        qb = sb("rqb", [128, 2, 512], BF16); kb = sb("rkb", [128, 2, 512], BF16); qd = sb("rqd", [128, 2, 512], BF16)
        vT = sb("rvT", [128, 512])
        V = sb("rV", [128, 4, 512], BF16); Kh = sb("rKh", [128, 4, 256], BF16)
        AmT = sb("rAmT", [128, 128], BF16)
        oT = sb("roT", [128, 4, 512]); sq = sb("rsq", [128, 4, 512]); of = sq
        mean = sb("rmean", [128, 512]); rstd = sb("rrstd", [128, 512]); gT = sb("rgT", [128, 512]); zb = sb("rzb", [128, 512], BF16)
        UTv = K.UT.rearrange("(k p) t -> p k t", p=128)

        def proj(dst_ps, col0, uu, n):
            for k in range(8):
                P.mm(dst_ps[:, :n], win[:, k, col0:col0 + 128], uu[:, k, :n], start=(k == 0), stop=(k == 7))

        it = 0
        for d in range(2):
            order = BLOCKS if d == 0 else ([BLOCKS[0]] + BLOCKS[:0:-1])
            P.memset(S[:], 0.0); P.memset(Sb[:], 0.0)
            for (s0, n, isctx) in order:
                uu = u[it % 2]; it += 1
                P.dma(uu[:, :, :n], UTv[:, :, s0:s0 + n])
                nch = n // C
                if not isctx:
                    P.dma(cs[:, :n], K.rope_cos[:, s0 - NCTX:s0 - NCTX + n])
                    P.dma(sn[:, :n], K.rope_sin[:, s0 - NCTX:s0 - NCTX + n])
                for h in range(4):
                    dh = d * 4 + h
                    for kt in range(2):
                        proj(ps[kt], h * 256 + kt * 128, uu, n)
                        P.copy(qT[:, kt, :n], ps[kt][:, :n], eng="scalar")
                    for kt in range(2):
                        proj(ps[kt], D + h * 256 + kt * 128, uu, n)
                        P.act(kT[:, kt, :n], ps[kt][:, :n], AF.Copy, scale=1.0 / 16.0)
                    if isctx:
                        zq, zk = qT, kT
                    else:
                        for (src, dst) in ((qT, qR), (kT, kR)):
                            P.tt(r1[:, :n], src[:, 0, :n], cs[:, :n], ALU.mult)
                            P.tt(r2[:, :n], src[:, 1, :n], sn[:, :n], ALU.mult, eng="gpsimd")
                            P.tt(dst[:, 0, :n], r1[:, :n], r2[:, :n], ALU.subtract)
                            P.tt(r1[:, :n], src[:, 1, :n], cs[:, :n], ALU.mult)
                            P.tt(r2[:, :n], src[:, 0, :n], sn[:, :n], ALU.mult, eng="gpsimd")
                            P.tt(dst[:, 1, :n], r1[:, :n], r2[:, :n], ALU.add)
                        zq, zk = qR, kR
                    P.copy(qb[:, :, :n], zq[:, :, :n], eng="gpsimd")
                    P.copy(kb[:, :, :n], zk[:, :, :n], eng="gpsimd")
                    for kt in range(2):
                        P.tt(qd[:, kt, :n].rearrange("p (c t) -> p c t", t=C), zq[:, kt, :n].rearrange("p (c t) -> p c t", t=C),
                             QD[:, dh, :].unsqueeze(1).to_broadcast([128, nch, C]), ALU.mult)
                    for vt in range(4):
                        proj(ps[vt % 2], 2 * D + h * 512 + vt * 128, uu, n)
                        P.copy(vT[:, :n], ps[vt % 2][:, :n], eng="scalar")
                        for ci in range(nch):
                            P.transpose(ps[2][:, ci * 128:(ci + 1) * 128], vT[:, ci * 128:(ci + 1) * 128], K.ident[:])
                        P.copy(V[:, :nch, vt * 128:(vt + 1) * 128], ps[2][:, :n].rearrange("p (c t) -> p c t", t=128), eng="scalar")
                    for kt in range(2):
                        for ci in range(nch):
                            P.transpose(ps[2][:, ci * 128:(ci + 1) * 128], zk[:, kt, ci * 128:(ci + 1) * 128], K.ident[:])
                        P.ts(Kh[:, :nch, kt * 128:(kt + 1) * 128], ps[2][:, :n].rearrange("p (c t) -> p c t", t=128),
                             kdc[:, dh:dh + 1], None, ALU.mult)
                    chs = list(range(nch)) if d == 0 else list(range(nch - 1, -1, -1))
                    for ci in chs:
                        cc = slice(ci * C, (ci + 1) * C)
                        for kt in range(2):
                            P.mm(ps[3][:, 0:128], kb[:, kt, cc], qb[:, kt, cc], start=(kt == 0), stop=(kt == 1))
                        P.tt(AmT[:], ps[3][:, 0:128], Dm[:, dh, :], ALU.mult)
                        for vt in range(4):
                            po = ps[4][:, vt * 128:(vt + 1) * 128]
                            P.mm(po, V[:, ci, vt * 128:(vt + 1) * 128], AmT[:], start=True, stop=False)
                            P.mm(po, Sb[:, h, 0, vt * 128:(vt + 1) * 128], qd[:, 0, cc], start=False, stop=False)
                            P.mm(po, Sb[:, h, 1, vt * 128:(vt + 1) * 128], qd[:, 1, cc], start=False, stop=True)
                        P.copy(oT[:, :, cc], ps[4][:, :].rearrange("p (v t) -> p v t", t=128), eng="scalar")
                        for kt in range(2):
                            P.mm(ps[5 + kt][:, :], Kh[:, ci, kt * 128:(kt + 1) * 128], V[:, ci, :])
                            P.stt(S[:, h, kt, :], S[:, h, kt, :], g128[:, dh:dh + 1], ps[5 + kt][:, :], ALU.mult, ALU.add)
                            P.copy(Sb[:, h, kt, :], S[:, h, kt, :], eng="gpsimd")
                    rows = lambda vt: slice(h * 512 + vt * 128, h * 512 + (vt + 1) * 128)
                    if d == 0:
                        for vt in range(4):
                            P.dma(OF[rows(vt), s0:s0 + n], oT[:, vt, :n])
                    else:
                        for vt in range(4):
                            P.dma(of[:, vt, :n], OF[rows(vt), s0:s0 + n])
                        P.tt(oT[:, :, :n], oT[:, :, :n], of[:, :, :n], ALU.add)
                        P.act(sq[:, :, :n], oT[:, :, :n], AF.Square)
                        for vt in range(4):
                            P.mm(ps[7][:, :n], ones[:], oT[:, vt, :n], start=(vt == 0), stop=(vt == 3))
                        for vt in range(4):
                            P.mm(ps[3][:, :n], ones[:], sq[:, vt, :n], start=(vt == 0), stop=(vt == 3))
                        P.act(mean[:, :n], ps[7][:, :n], AF.Copy, scale=1.0 / 512)
                        P.tt(rstd[:, :n], mean[:, :n], mean[:, :n], ALU.mult)
                        P.stt(rstd[:, :n], ps[3][:, :n], 1.0 / 512, rstd[:, :n], ALU.mult, ALU.subtract)
                        P.ts(rstd[:, :n], rstd[:, :n], LN_EPS, None, ALU.add)
                        P.act(rstd[:, :n], rstd[:, :n], AF.Sqrt)
                        P._add("vector", lambda e, a=rstd[:, :n]: e.reciprocal(a, a), [rstd[:, :n]], [rstd[:, :n]])
                        for vt in range(4):
                            gi = h * 4 + vt
                            P.tt(sq[:, vt, :n], oT[:, vt, :n], mean[:, :n], ALU.subtract)
                            P.tt(sq[:, vt, :n], sq[:, vt, :n], rstd[:, :n], ALU.mult)
                            P.ts(sq[:, vt, :n], sq[:, vt, :n], gng[:, gi:gi + 1], gnb[:, gi:gi + 1], ALU.mult, ALU.add)
                            proj(ps[vt % 2], 4 * D + h * 512 + vt * 128, uu, n)
                            P.act(gT[:, :n], ps[vt % 2][:, :n], AF.Silu)
                            P.tt(zb[:, :n], sq[:, vt, :n], gT[:, :n], ALU.mult)
                            P.dma(K.ZT[rows(vt), s0:s0 + n], zb[:, :n])
    P.barrier()
    return 2 * D


def mixer_rwkv(K, l):
    nc, P, ps = K.nc, K.P, K.ps
    j = l // 4
    OF = K.scrA; BN = K.scrB
    C = 64
    DS = 0.6065306597126334
    with ExitStack() as st:
        def sb(name, shape, dt=F32):
            K.uid = getattr(K, "uid", 0) + 1
            return st.enter_context(nc.sbuf_tensor("%s_%d" % (name, K.uid), shape, dt))
        W3 = sb("wW3", [128, 3, 8, D], BF16)
        for i in range(3):
            P.dma(W3[:, i], K.rwkv_w_in[j, i].rearrange("(k p) n -> p k n", p=128), eng="gpsimd")
        wl1 = sb("wwl1", [128, 2, 8, 64], BF16); al1 = sb("wal1", [128, 2, 8, 64], BF16); gl1 = sb("wgl1", [128, 8, 128], BF16)
        wl2 = sb("wwl2", [64, 2, D], BF16); al2 = sb("wal2", [64, 2, D], BF16); gl2 = sb("wgl2", [128, D], BF16)
        for z in range(2):
            P.dma(wl1[:, z], K.rwkv_w_l1[j, z].rearrange("(k p) n -> p k n", p=128), eng="gpsimd")
            P.dma(al1[:, z], K.rwkv_a_l1[j, z].rearrange("(k p) n -> p k n", p=128), eng="gpsimd")
            P.dma(wl2[:, z], K.rwkv_w_l2[j, z], eng="gpsimd")
            P.dma(al2[:, z], K.rwkv_a_l2[j, z], eng="gpsimd")
        P.dma(gl1[:], K.rwkv_g_l1[j].rearrange("(k p) n -> p k n", p=128), eng="gpsimd")
        P.dma(gl2[:], K.rwkv_g_l2[j], eng="gpsimd")
        vec = sb("wvec", [128, 16, 8]); P.dma(vec[:], K.rwkv_vec[j])
        omm = sb("womm", [128, 6, 8]); P.ts(omm[:], vec[:, 0:6, :], -1.0, 1.0, ALU.mult, ALU.add)
        P.ts(vec[:, 15, :], vec[:, 15, :], -1.0, 1.0, ALU.mult, ALU.add)
        cst = sb("wcst", [128, 5, 128]); P.dma(cst[:], K.rw_consts)
        bones = cst[:, 4, :]
        nst = sb("wnst", [128, 2, 512]); P.memset(nst[:], 1.0)
        P.memset(nst[:, 0, 0:512:C], 0.0); P.memset(nst[:, 1, C - 1:512:C], 0.0)
        ue = sb("wue", [128, 8, 514], BF16); xm = sb("wxm", [128, 4, 8, 512], BF16); tq = sb("wtq", [128, 512], BF16)
        hw = sb("whw", [64, 512], BF16); ha = sb("wha", [64, 512], BF16); hg = sb("whg", [128, 512], BF16)
        F = lambda nm: sb(nm, [128, 512])
        rT = F("wrT"); kT = F("wkT"); vT = F("wvT"); lw = F("wlw"); aT = F("waT"); kk = F("wkk"); ka = F("wka"); kt = F("wkt")
        B = F("wB"); e1 = F("we1"); e2 = F("we2"); t1 = F("wt1"); oT = F("woT"); of = F("wof"); bn = F("wbn"); gT = F("wgT")
        zb = sb("wzb", [128, 512], BF16)
        names = ["kap", "bt", "ktl", "rt", "Kh", "Bh", "Vb"]
        bd = {nm: sb("wbd" + nm, [128, 8, 128]) for nm in names}
        for nm in names:
            P.memset(bd[nm][:], 0.0)
        tok = {nm: sb("wtok" + nm, [128, 8, 128]) for nm in ("V", "Kh", "Bh")}
        eBl = sb("weBl", [128, 8])
        Sk = sb("wSk", [128, 8, 128])
        Nm = [[sb("wN%d%d" % (q, i), [128, 128]) for i in range(2)] for q in range(2)]
        Nt = [[sb("wNt%d%d" % (q, i), [128, 128]) for i in range(2)] for q in range(2)]
        Tt = [[sb("wTt%d%d" % (q, i), [128, 128]) for i in range(2)] for q in range(2)]
        NkT = [sb("wNkT%d" % q, [128, 128]) for q in range(2)]; MkT = [sb("wMkT%d" % q, [128, 128]) for q in range(2)]
        MbT = [sb("wMbT%d" % q, [128, 128]) for q in range(2)]
        RH = sb("wRH", [128, 128]); nU = sb("wnU", [128, 128])
        UTv = K.UT.rearrange("(k p) t -> p k t", p=128)

        def projW(dst_ps, i, col0, n):
            for k in range(8):
                P.mm(dst_ps[:, :n], W3[:, i, k, col0:col0 + 128], xm[:, i, k, :n], start=(k == 0), stop=(k == 7))

        def bsum(dst_ps, src, n):
            P.mm(dst_ps[:, :n], bones, src, start=True, stop=True)

        for z in range(2):
            order = BLOCKS if z == 0 else ([BLOCKS[0]] + BLOCKS[:0:-1])
            P.memset(Sk[:], 0.0)
            for (s0, n, isctx) in order:
                nch = n // C
                seg0, seg1 = (0, NCTX) if isctx else (NCTX, NT)
                P.memset(ue[:, :, 0:1], 0.0); P.memset(ue[:, :, n + 1:n + 2], 0.0)
                P.dma(ue[:, :, 1:n + 1], UTv[:, :, s0:s0 + n])
                if s0 > seg0:
                    P.dma(ue[:, :, 0:1], UTv[:, :, s0 - 1:s0], allow_slow_non_contiguous=True)
                if s0 + n < seg1:
                    P.dma(ue[:, :, n + 1:n + 2], UTv[:, :, s0 + n:s0 + n + 1], allow_slow_non_contiguous=True)
                def mix(i, slot):
                    for k in range(8):
                        shv = ue[:, k, 0:n] if k < 4 else ue[:, k, 2:n + 2]
                        P.ts(tq[:, :n], ue[:, k, 1:n + 1], omm[:, i, k:k + 1], None, ALU.mult, eng="gpsimd")
                        P.stt(xm[:, slot, k, :n], shv, vec[:, i, k:k + 1], tq[:, :n], ALU.mult, ALU.add)
                for i in range(3):
                    mix(i, i)
                mix(3, 3)
                for k in range(8):
                    P.mm(ps[0][0:64, :n], wl1[:, z, k, :], xm[:, 3, k, :n], start=(k == 0), stop=(k == 7))
                P.act(hw[:, :n], ps[0][0:64, :n], AF.Tanh)
                mix(4, 3)
                for k in range(8):
                    P.mm(ps[1][0:64, :n], al1[:, z, k, :], xm[:, 3, k, :n], start=(k == 0), stop=(k == 7))
                P.copy(ha[:, :n], ps[1][0:64, :n], eng="scalar")
                if z == 1:
                    mix(5, 3)
                    for k in range(8):
                        P.mm(ps[0][:, :n], gl1[:, k, :], xm[:, 3, k, :n], start=(k == 0), stop=(k == 7))
                    P.act(hg[:, :n], ps[0][:, :n], AF.Sigmoid)
                for pt in range(8):
                    c0 = pt * 128
                    projW(ps[0], 0, c0, n); P.copy(rT[:, :n], ps[0][:, :n], eng="scalar")
                    projW(ps[1], 1, c0, n); P.copy(kT[:, :n], ps[1][:, :n], eng="scalar")
                    projW(ps[0], 2, c0, n); P.copy(vT[:, :n], ps[0][:, :n], eng="scalar")
                    P.mm(ps[1][:, :n], wl2[:, z, c0:c0 + 128], hw[:, :n])
                    P.act(lw[:, :n], ps[1][:, :n], AF.Sigmoid, bias=vec[:, 6 + z, pt:pt + 1])
                    P.ts(lw[:, :n], lw[:, :n], -DS, None, ALU.mult)
                    P.mm(ps[0][:, :n], al2[:, z, c0:c0 + 128], ha[:, :n])
                    P.act(aT[:, :n], ps[0][:, :n], AF.Sigmoid, bias=vec[:, 8 + z, pt:pt + 1])
                    P.ts(kk[:, :n], kT[:, :n], vec[:, 10, pt:pt + 1], None, ALU.mult)
                    P.tt(t1[:, :n], kk[:, :n], kk[:, :n], ALU.mult)
                    bsum(ps[2], t1[:, :n], n)
                    P.ts(t1[:, :n], ps[2][:, :n], 1e-12, None, ALU.add)
                    P.act(t1[:, :n], t1[:, :n], AF.Sqrt)
                    P._add("vector", lambda e, a=t1[:, :n]: e.reciprocal(a, a), [t1[:, :n]], [t1[:, :n]])
                    P.tt(kk[:, :n], kk[:, :n], t1[:, :n], ALU.mult)
                    P.tt(ka[:, :n], kk[:, :n], aT[:, :n], ALU.mult)
                    P.ts(t1[:, :n], aT[:, :n], vec[:, 11, pt:pt + 1], vec[:, 15, pt:pt + 1], ALU.mult, ALU.add)
                    P.tt(kt[:, :n], kT[:, :n], t1[:, :n], ALU.mult)
                    P.stt(t1[:, :n], rT[:, :n], vec[:, 12, pt:pt + 1], kt[:, :n], ALU.mult, ALU.mult)
                    bsum(ps[2], t1[:, :n], n)
                    P.tt(bn[:, :n], ps[2][:, :n], vT[:, :n], ALU.mult)
                    if z == 0:
                        P.scan(B[:, :n], nst[:, 0, :n], lw[:, :n], 0.0, ALU.mult, ALU.add)
                    else:
                        P.scan(B[:, n - 1::-1], nst[:, 1, n - 1::-1], lw[:, n - 1::-1], 0.0, ALU.mult, ALU.add)
                    cv = lambda ap, lo: ap[lo:lo + 64, :n].rearrange("p (c t) -> p c t", t=C)
                    def to_bd(nm, a_, b_):
                        for hh in range(2):
                            lo = hh * 64
                            P.tt(bd[nm][lo:lo + 64, :nch, lo:lo + 64], cv(a_, lo), cv(b_, lo), ALU.mult)
                    P.tt(e1[:, :n], B[:, :n], lw[:, :n], ALU.subtract)
                    P.act(e1[:, :n], e1[:, :n], AF.Exp)
                    to_bd("kap", kk, e1)
                    P.act(e1[:, :n], B[:, :n], AF.Exp)
                    to_bd("rt", rT, e1)
                    P.act(e1[:, :n], B[:, :n], AF.Exp, scale=-1.0)
                    to_bd("bt", ka, e1)
                    to_bd("ktl", kt, e1)
                    Bv = B[:, :n].rearrange("p (c t) -> p c t", t=C)
                    last = C - 1 if z == 0 else 0
                    P.act(eBl[:, :nch], Bv[:, :, last], AF.Exp)
                    P.tt(e2[:, :n].rearrange("p (c t) -> p c t", t=C), Bv, Bv[:, :, last:last + 1].to_broadcast([128, nch, C]), ALU.subtract)
                    P.act(e2[:, :n], e2[:, :n], AF.Exp, scale=-1.0)
                    to_bd("Kh", kt, e2)
                    to_bd("Bh", ka, e2)
                    for hh in range(2):
                        lo = hh * 64
                        P.copy(bd["Vb"][lo:lo + 64, :nch, lo:lo + 64], cv(vT, lo), eng="gpsimd")
                    for nm, src in (("V", "Vb"), ("Kh", "Kh"), ("Bh", "Bh")):
                        for g4 in range(0, nch, 4):
                            for c in range(g4, g4 + 4):
                                P.transpose(ps[3][:, (c - g4) * 128:(c - g4 + 1) * 128], bd[src][:, c, :], K.ident[:])
                            P.copy(tok[nm][:, g4:g4 + 4, :].rearrange("p c t -> p (c t)"), ps[3][:, :], eng="scalar")
                    sl, su, ui, li = (cst[:, i, :] for i in range(4))
                    m_strict_ti = sl if z == 0 else su
                    m_strict_it = su if z == 0 else sl
                    m_incl_it = ui if z == 0 else li
                    chs = list(range(nch)) if z == 0 else list(range(nch - 1, -1, -1))
                    for gi2 in range(0, len(chs), 2):
                        grp = chs[gi2:gi2 + 2]
                        for q, c in enumerate(grp):
                            kap = bd["kap"][:, c, :]; bt = bd["bt"][:, c, :]; ktl = bd["ktl"][:, c, :]; rt_ = bd["rt"][:, c, :]
                            psI = ps[5 + q]
                            P.mm(ps[4][:, 0:128], kap, bt)
                            P.mm(ps[4][:, 128:256], bt, kap)
                            P.mm(ps[4][:, 256:384], ktl, kap)
                            P.mm(ps[4][:, 384:512], ktl, rt_)
                            P.mm(psI[:, 384:512], bt, rt_)
                            P.tt(Nm[q][0][:], ps[4][:, 0:128], m_strict_ti, ALU.mult)
                            P.tt(Nt[q][0][:], ps[4][:, 128:256], m_strict_it, ALU.mult)
                            P.tt(NkT[q][:], ps[4][:, 256:384], m_strict_it, ALU.mult)
                            P.tt(MkT[q][:], ps[4][:, 384:512], m_incl_it, ALU.mult)
                            P.tt(MbT[q][:], psI[:, 384:512], m_incl_it, ALU.mult)
                            P.tt(Tt[q][0][:], K.ident[:], Nt[q][0][:], ALU.subtract)
                        cur = 0
                        for lvl in range(5):
                            for q, c in enumerate(grp):
                                psI = ps[5 + q]
                                X, Xt, T_ = Nm[q][cur], Nt[q][cur], Tt[q][cur]
                                X2, Xt2, T2 = Nm[q][1 - cur], Nt[q][1 - cur], Tt[q][1 - cur]
                                P.mm(psI[:, 0:128], Xt[:], X[:])
                                if lvl < 4:
                                    P.mm(psI[:, 128:256], X[:], Xt[:])
                                P.copy(X2[:], psI[:, 0:128], eng="scalar")
                                if lvl < 4:
                                    P.copy(Xt2[:], psI[:, 128:256], eng="scalar")
                                P.mm(psI[:, 256:384], X2[:], T_[:])
                                P.tt(T2[:], psI[:, 256:384], T_[:], ALU.add)
                            cur = 1 - cur
                        for q, c in enumerate(grp):
                            kap = bd["kap"][:, c, :]; rt_ = bd["rt"][:, c, :]
                            TT = Tt[q][cur]
                            P.mm(ps[7][:, 0:128], kap, Sk[:, pt, :], start=True, stop=False)
                            P.mm(ps[7][:, 0:128], NkT[q][:], tok["V"][:, c, :], start=False, stop=True)
                            P.copy(RH[:], ps[7][:, 0:128], eng="scalar")
                            P.mm(ps[7][:, 128:256], TT[:], RH[:])
                            P.act(nU[:], ps[7][:, 128:256], AF.Copy, scale=-1.0)
                            P.mm(ps[7][:, 256:384], Sk[:, pt, :], rt_, start=True, stop=False)
                            P.mm(ps[7][:, 256:384], tok["V"][:, c, :], MkT[q][:], start=False, stop=False)
                            P.mm(ps[7][:, 256:384], nU[:], MbT[q][:], start=False, stop=True)
                            for hh in range(2):
                                lo = hh * 64
                                P.copy(oT[lo:lo + 64, c * C:(c + 1) * C], ps[7][lo:lo + 64, 256 + lo:256 + lo + 64], eng="scalar")
                            P.mm(ps[7][:, 384:512], tok["Kh"][:, c, :], tok["V"][:, c, :], start=True, stop=False)
                            P.mm(ps[7][:, 384:512], tok["Bh"][:, c, :], nU[:], start=False, stop=True)
                            P.stt(Sk[:, pt, :], Sk[:, pt, :], eBl[:, c:c + 1], ps[7][:, 384:512], ALU.mult, ALU.add)
                    rows = slice(c0, c0 + 128)
                    if z == 0:
                        P.dma(OF[rows, s0:s0 + n], oT[:, :n])
                        P.dma(BN[rows, s0:s0 + n], bn[:, :n])
                    else:
                        P.dma(of[:, :n], OF[rows, s0:s0 + n])
                        P.tt(oT[:, :n], oT[:, :n], of[:, :n], ALU.add)
                        P.dma(of[:, :n], BN[rows, s0:s0 + n])
                        P.tt(bn[:, :n], bn[:, :n], of[:, :n], ALU.add)
                        bsum(ps[2], oT[:, :n], n)
                        P.act(e1[:, :n], ps[2][:, :n], AF.Copy, scale=1.0 / 64)
                        P.tt(oT[:, :n], oT[:, :n], e1[:, :n], ALU.subtract)
                        P.tt(t1[:, :n], oT[:, :n], oT[:, :n], ALU.mult)
                        bsum(ps[2], t1[:, :n], n)
                        P.ts(t1[:, :n], ps[2][:, :n], 1.0 / 64, 64e-5, ALU.mult, ALU.add)
                        P.act(t1[:, :n], t1[:, :n], AF.Sqrt)
                        P._add("vector", lambda e, a=t1[:, :n]: e.reciprocal(a, a), [t1[:, :n]], [t1[:, :n]])
                        P.tt(oT[:, :n], oT[:, :n], t1[:, :n], ALU.mult)
                        P.ts(oT[:, :n], oT[:, :n], vec[:, 13, pt:pt + 1], vec[:, 14, pt:pt + 1], ALU.mult, ALU.add)
                        P.tt(oT[:, :n], oT[:, :n], bn[:, :n], ALU.add)
                        P.mm(ps[0][:, :n], gl2[:, c0:c0 + 128], hg[:, :n])
                        P.tt(zb[:, :n], oT[:, :n], ps[0][:, :n], ALU.mult)
                        P.dma(K.ZT[rows, s0:s0 + n], zb[:, :n])
    P.barrier()
    return D
```
